# Optimizing a Trainium2 kernel written in Bass

```python
import jax, jax.numpy as jnp
from jax import lax
import numpy as np

D_MODEL = 1024
BATCH = 8
SEQ = 2048
DEPTH = 2

MLSTM_HEADS = 4
MLSTM_HEAD_DIM = 256
MLSTM_WIDTH = MLSTM_HEADS * MLSTM_HEAD_DIM
MLSTM_CHUNK = 128
CONV_WIDTH = 4
POOL_WINDOWS = (2, 4, 8, 16)
POOL_GROUPS = 4
POOL_GROUP_DIM = 128
POOL_WIDTH = POOL_GROUPS * POOL_GROUP_DIM
N_BRANCH = 2
SPLIT_POINTS = (
    MLSTM_WIDTH,
    2 * MLSTM_WIDTH,
    3 * MLSTM_WIDTH,
    4 * MLSTM_WIDTH,
    4 * MLSTM_WIDTH + MLSTM_HEADS,
    4 * MLSTM_WIDTH + 2 * MLSTM_HEADS,
    4 * MLSTM_WIDTH + 2 * MLSTM_HEADS + POOL_WIDTH,
)
N_IN = 4 * MLSTM_WIDTH + 2 * MLSTM_HEADS + POOL_WIDTH + N_BRANCH * D_MODEL
D_FF_DENSE = 2816
N_EXPERTS = 8
TOP_K = 2
D_FF_EXPERT = 3584
N_DENSE = (DEPTH + 1) // 2
N_MOE = DEPTH // 2
EPS = 1e-6

kernel_name = "hybrid_mlstm_pool_moe_adaln"


def rmsnorm(x, g):
    xf = x.astype(jnp.float32)
    y = xf * lax.rsqrt(jnp.mean(xf * xf, axis=-1, keepdims=True) + EPS)
    return (y * g.astype(jnp.float32)).astype(x.dtype)


def modulate(h, shift, scale):
    return h * (1 + scale[:, None, :]) + shift[:, None, :]


def causal_depthwise_conv(x, w):
    return lax.conv_general_dilated(
        x, w[:, None, :].astype(x.dtype), window_strides=(1,),
        padding=((CONV_WIDTH - 1, 0),), dimension_numbers=('NWC', 'WIO', 'NWC'),
        feature_group_count=x.shape[-1])


def mlstm_chunkwise(q, k, v, i_pre, f_pre):
    bsz, nh, t_len, dh = q.shape
    L = MLSTM_CHUNK
    nc = t_len // L
    f32 = jnp.float32
    q = q.astype(f32).reshape(bsz, nh, nc, L, dh) * (dh ** -0.5)
    k = k.astype(f32).reshape(bsz, nh, nc, L, dh)
    v = v.astype(f32).reshape(bsz, nh, nc, L, dh)
    log_f = jax.nn.log_sigmoid(f_pre.astype(f32)).reshape(bsz, nh, nc, L)
    log_i = i_pre.astype(f32).reshape(bsz, nh, nc, L)
    b = jnp.cumsum(log_f, axis=-1)
    b_tot = b[..., -1]
    a = b_tot[..., None] - b + log_i
    m_loc = jnp.max(a, axis=-1)
    w = jnp.exp(a - m_loc[..., None])
    c_loc = jnp.einsum('bhclv,bhclk->bhcvk', w[..., None] * v, k)
    n_loc = jnp.einsum('bhcl,bhclk->bhck', w, k)

    def step(carry, xs):
        c_st, n_st, m_st = carry
        bt, ml, cl, nl = xs
        m_new = jnp.maximum(bt + m_st, ml)
        s_old = jnp.exp(bt + m_st - m_new)
        s_loc = jnp.exp(ml - m_new)
        c_new = s_old[..., None, None] * c_st + s_loc[..., None, None] * cl
        n_new = s_old[..., None] * n_st + s_loc[..., None] * nl
        return (c_new, n_new, m_new), (c_st, n_st, m_st)

    init = (jnp.zeros((bsz, nh, dh, dh), f32), jnp.zeros((bsz, nh, dh), f32), jnp.zeros((bsz, nh), f32))
    xs = (jnp.moveaxis(b_tot, 2, 0), jnp.moveaxis(m_loc, 2, 0),
          jnp.moveaxis(c_loc, 2, 0), jnp.moveaxis(n_loc, 2, 0))
    _, (c_prev, n_prev, m_prev) = lax.scan(step, init, xs)
    c_prev = jnp.moveaxis(c_prev, 0, 2)
    n_prev = jnp.moveaxis(n_prev, 0, 2)
    m_prev = jnp.moveaxis(m_prev, 0, 2)

    causal = jnp.tril(jnp.ones((L, L), dtype=bool))
    d = jnp.where(causal, b[..., :, None] - b[..., None, :] + log_i[..., None, :], -jnp.inf)
    inter_log = b + m_prev[..., None]
    m_comb = jnp.maximum(inter_log, jnp.max(d, axis=-1))
    s = jnp.einsum('bhctd,bhcsd->bhcts', q, k) * jnp.exp(d - m_comb[..., None])
    w_inter = jnp.exp(inter_log - m_comb)
    num = (jnp.einsum('bhcts,bhcsd->bhctd', s, v)
           + w_inter[..., None] * jnp.einsum('bhcvk,bhctk->bhctv', c_prev, q))
    den = jnp.sum(s, axis=-1) + w_inter * jnp.einsum('bhck,bhctk->bhct', n_prev, q)
    den = jnp.maximum(jnp.abs(den), jnp.exp(-m_comb))
    h = num / den[..., None]
    return h.reshape(bsz, nh, t_len, dh)


def multiscale_pool(u):
    bsz, t_len, _ = u.shape
    uf = u.astype(jnp.float32).reshape(bsz, t_len, POOL_GROUPS, POOL_GROUP_DIM)
    cs = jnp.pad(jnp.cumsum(uf, axis=1), ((0, 0), (1, 0), (0, 0), (0, 0)))
    t = jnp.arange(t_len)[:, None]
    win = jnp.asarray(np.array(POOL_WINDOWS, dtype=np.int32))[None, :]
    lo = jnp.maximum(t + 1 - win, 0)
    g_idx = jnp.arange(POOL_GROUPS)[None, :]
    window_sum = cs[:, 1:] - cs[:, lo, g_idx]
    count = (t + 1 - lo).astype(jnp.float32)
    return window_sum / count[None, :, :, None] - uf


def token_mixer(h, w_in, conv_w, i_bias, f_bias, head_gain, pool_w, pool_scale, proj_a, proj_b, w_out):
    bsz, t_len, _ = h.shape
    z = h @ w_in.astype(h.dtype)
    q, k, v, o, ig, fg, u, gates = jnp.split(z, SPLIT_POINTS, axis=-1)
    qk = jax.nn.silu(causal_depthwise_conv(jnp.concatenate([q, k], axis=-1), conv_w))
    q, k = jnp.split(qk, 2, axis=-1)
    to_heads = lambda a: a.reshape(bsz, t_len, MLSTM_HEADS, MLSTM_HEAD_DIM).transpose(0, 2, 1, 3)
    h_a = mlstm_chunkwise(to_heads(q), to_heads(k), to_heads(v),
                          (ig + i_bias).transpose(0, 2, 1), (fg + f_bias).transpose(0, 2, 1))
    h_a = h_a * lax.rsqrt(jnp.mean(h_a * h_a, axis=-1, keepdims=True) + EPS)
    h_a = h_a.transpose(0, 2, 1, 3).reshape(bsz, t_len, MLSTM_WIDTH).astype(h.dtype)
    h_a = h_a * head_gain * jax.nn.sigmoid(o)
    pooled = multiscale_pool(u).astype(h.dtype)
    h_b = jnp.einsum('btgc,gcd->btgd', pooled, pool_w).reshape(bsz, t_len, POOL_WIDTH) * pool_scale
    gate_a, gate_b = jnp.split(jax.nn.sigmoid(gates), N_BRANCH, axis=-1)
    merged = gate_a * (h_a @ proj_a) + gate_b * (h_b @ proj_b)
    return merged @ w_out


def swiglu(h, w_gate, w_up, w_down):
    return (jax.nn.silu(h @ w_gate) * (h @ w_up)) @ w_down


def moe_swiglu(h, router_w, router_b, w_gate, w_up, w_down):
    logits = (h @ router_w).astype(jnp.float32) + router_b.astype(jnp.float32)
    top_val, top_idx = lax.top_k(logits, TOP_K)
    top_w = jax.nn.softmax(top_val, axis=-1)
    combine = jnp.sum(jax.nn.one_hot(top_idx, N_EXPERTS, dtype=jnp.float32) * top_w[..., None], axis=-2)
    combine = combine.astype(h.dtype)
    out = jnp.zeros_like(h)
    for e in range(N_EXPERTS):
        out = out + combine[..., e:e + 1] * swiglu(h, w_gate[e], w_up[e], w_down[e])
    return out


def setup_inputs(seed: int = 0) -> dict:
    key = jax.random.key(seed)
    ks = jax.random.split(key, 32)
    f32 = jnp.float32
    D, W, H, P = D_MODEL, MLSTM_WIDTH, MLSTM_HEADS, POOL_WIDTH
    nrm = lambda kk, shape, fan_in: jax.random.normal(kk, shape, f32) * (fan_in ** -0.5)
    noise = lambda kk, shape, s: s * jax.random.normal(kk, shape, f32)
    return {
        "x": jax.random.normal(ks[0], (BATCH, SEQ, D), f32),
        "c": jax.random.normal(ks[1], (BATCH, D), f32),
        "norm_mix": 1.0 + noise(ks[2], (DEPTH, D), 0.05),
        "norm_ffn": 1.0 + noise(ks[3], (DEPTH, D), 0.05),
        "w_ada": 0.5 * nrm(ks[4], (DEPTH, D, 6 * D), D),
        "b_ada": noise(ks[5], (DEPTH, 6 * D), 0.02),
        "w_in": nrm(ks[6], (DEPTH, D, N_IN), D),
        "conv_w": nrm(ks[7], (DEPTH, CONV_WIDTH, 2 * W), CONV_WIDTH),
        "i_bias": noise(ks[8], (DEPTH, H), 0.1),
        "f_bias": jnp.linspace(3.0, 6.0, H, dtype=f32)[None, :] + noise(ks[9], (DEPTH, H), 0.1),
        "head_gain": 1.0 + noise(ks[10], (DEPTH, W), 0.05),
        "pool_w": nrm(ks[11], (DEPTH, POOL_GROUPS, POOL_GROUP_DIM, POOL_GROUP_DIM), POOL_GROUP_DIM),
        "pool_scale": 1.0 + noise(ks[12], (DEPTH, P), 0.05),
        "proj_a": nrm(ks[13], (DEPTH, W, D), W),
        "proj_b": nrm(ks[14], (DEPTH, P, D), P),
        "w_out": nrm(ks[15], (DEPTH, D, D), D),
        "ffn_w_gate": nrm(ks[16], (N_DENSE, D, D_FF_DENSE), D),
        "ffn_w_up": nrm(ks[17], (N_DENSE, D, D_FF_DENSE), D),
        "ffn_w_down": nrm(ks[18], (N_DENSE, D_FF_DENSE, D), D_FF_DENSE),
        "router_w": nrm(ks[19], (N_MOE, D, N_EXPERTS), D),
        "router_b": noise(ks[20], (N_MOE, N_EXPERTS), 0.01),
        "moe_w_gate": nrm(ks[21], (N_MOE, N_EXPERTS, D, D_FF_EXPERT), D),
        "moe_w_up": nrm(ks[22], (N_MOE, N_EXPERTS, D, D_FF_EXPERT), D),
        "moe_w_down": nrm(ks[23], (N_MOE, N_EXPERTS, D_FF_EXPERT, D), D_FF_EXPERT),
        "final_norm": 1.0 + noise(ks[24], (D,), 0.05),
    }


def reference(x, c, norm_mix, norm_ffn, w_ada, b_ada, w_in, conv_w, i_bias, f_bias, head_gain,
              pool_w, pool_scale, proj_a, proj_b, w_out, ffn_w_gate, ffn_w_up, ffn_w_down,
              router_w, router_b, moe_w_gate, moe_w_up, moe_w_down, final_norm):
    c_act = jax.nn.silu(c)
    for l in range(DEPTH):
        mod = c_act @ w_ada[l] + b_ada[l]
        sh1, sc1, g1, sh2, sc2, g2 = jnp.split(mod, 6, axis=-1)
        h = modulate(rmsnorm(x, norm_mix[l]), sh1, sc1)
        x = x + g1[:, None, :] * token_mixer(h, w_in[l], conv_w[l], i_bias[l], f_bias[l], head_gain[l],
                                             pool_w[l], pool_scale[l], proj_a[l], proj_b[l], w_out[l])
        h = modulate(rmsnorm(x, norm_ffn[l]), sh2, sc2)
        j = l // 2
        if l % 2 == 0:
            f = swiglu(h, ffn_w_gate[j], ffn_w_up[j], ffn_w_down[j])
        else:
            f = moe_swiglu(h, router_w[j], router_b[j], moe_w_gate[j], moe_w_up[j], moe_w_down[j])
        x = x + g2[:, None, :] * f
    return rmsnorm(x, final_norm)
```

```python
import numpy as np
from contextlib import ExitStack
import concourse.bass as bass
import concourse.mybir as mybir
from concourse.bass_utils import run_bass_kernel_spmd

F32 = mybir.dt.float32
BF16 = mybir.dt.bfloat16
AF = mybir.ActivationFunctionType
ALU = mybir.AluOpType

PE, ACT, DVE, POOL, SP = "pe", "act", "dve", "pool", "sp"
ENGS = [PE, ACT, DVE, POOL, SP]

D = 1024
T = 2048
NP = 4
TB = 512
NCH = 4
EPS = 1e-6
N_IN = 6664
DFF = 2816
DFE = 3584
NEXP = 8
LC = 140
NCOLS = 2 * LC + 8

C_ID, C_MASK, C_ONES = 0, 128, 256
C_RMUL, C_RADD, C_INVC = 384, 896, 1408
NCONST = 1408 + 64


class Prog:
    def __init__(self, nc):
        self.nc = nc
        self.ops = {e: [] for e in ENGS}
        self.last_w = {}
        self.readers = {}
        self.chan_cnt = {}
        self.nbar = 0

    def _add(self, eng, fn, reads, writes, dma_chan=None):
        pr = tuple(k for k in reads if (isinstance(k, tuple) and k and k[0] == "ps") or k in ("psb0", "psb1"))
        writes = tuple(writes) + pr
        deps = set()
        for k in reads:
            for w in self.last_w.get(k, {}).values():
                deps.add(w)
        for k in writes:
            for w in self.last_w.get(k, {}).values():
                deps.add(w)
            for r in self.readers.get(k, {}).values():
                deps.add(r)
        idx = len(self.ops[eng])
        if dma_chan is not None:
            cum = self.chan_cnt.get(dma_chan, 0) + 16
            self.chan_cnt[dma_chan] = cum
            ref = ("d", dma_chan, cum)
            who = ("q", dma_chan)
        else:
            ref = ("c", eng, idx)
            who = eng
        fdeps = []
        for d in deps:
            if d[0] == "c" and d[1] == eng:
                if eng == PE or eng == SP:
                    continue
                fdeps.append(d)
            else:
                fdeps.append(d)
        self.ops[eng].append(dict(fn=fn, deps=fdeps, ref=ref, signal=False))
        for k in reads:
            self.readers.setdefault(k, {})[who] = ref
        for k in writes:
            self.last_w.setdefault(k, {})[who] = ref
            self.readers[k] = {}
        return ref

    def op(self, eng, fn, reads=(), writes=()):
        return self._add(eng, fn, tuple(reads), tuple(writes))

    def dma(self, eng, out, in_, reads=(), writes=(), chan=None):
        def fn(e, out=out, in_=in_):
            return e.dma_start(out=out, in_=in_)
        return self._add(eng, fn, tuple(reads), tuple(writes), dma_chan=chan)

    def emit(self, stack, final_waits=()):
        nc = self.nc
        for e in ENGS:
            for op in self.ops[e]:
                for d in op["deps"]:
                    if d[0] == "c":
                        self.ops[d[1]][d[2]]["signal"] = True
        sigcount = {}
        for e in ENGS:
            c = 0
            arr = []
            for op in self.ops[e]:
                if op["signal"]:
                    c += 1
                arr.append(c)
            sigcount[e] = arr
        esem = {e: stack.enter_context(nc.semaphore("s_" + e)) for e in ENGS}
        csem = {ch: stack.enter_context(nc.semaphore("d_%d" % i))
                for i, ch in enumerate(self.chan_cnt)}
        block = stack.enter_context(nc.Block())
        engobj = {PE: "tensor", ACT: "scalar", DVE: "vector", POOL: "gpsimd", SP: "sync"}

        def make(e):
            def body(eng):
                waited = {}
                for op in self.ops[e]:
                    for d in op["deps"]:
                        if d[0] == "c":
                            sem, val, key = esem[d[1]], sigcount[d[1]][d[2]], ("c", d[1])
                        else:
                            sem, val, key = csem[d[1]], d[2], ("d", d[1])
                        if waited.get(key, 0) >= val:
                            continue
                        waited[key] = val
                        eng.wait_ge(sem, val)
                    ins = op["fn"](eng)
                    if op["ref"][0] == "d":
                        ins.then_inc(csem[op["ref"][1]], 16)
                    elif op["signal"]:
                        ins.then_inc(esem[e], 1)
                if e == SP:
                    for ch in final_waits:
                        eng.wait_ge(csem[ch], self.chan_cnt[ch])
            return body

        for e in ENGS:
            getattr(block, engobj[e])(make(e))


class _Stop(Exception):
    pass


def build(nc, layers=(0, 1), final=True, dump=None, ffn=True, stage=99):
    st = ExitStack()
    P = Prog(nc)

    def dram(name, shape, kind="ExternalInput"):
        return nc.dram_tensor(name, list(shape), F32, kind=kind).ap()

    xT_d = dram("xT", [D, T])
    cT_d = dram("cT", [128, 8])
    cols_d = dram("cols", [128, NCOLS])
    gb_d = dram("gb", [4, 4])
    rb_d = dram("rb", [128, 8])
    consts_d = dram("consts", [128, NCONST])
    w_ada = dram("w_ada", [2, D, 6 * D])
    w_in = dram("w_in", [2, D, N_IN])
    pool_w = dram("pool_w", [2, 4, 128, 128])
    proj_a = dram("proj_a", [2, D, D])
    proj_b = dram("proj_b", [2, 512, D])
    w_out = dram("w_out", [2, D, D])
    ffn_g = dram("ffn_w_gate", [1, D, DFF])
    ffn_u = dram("ffn_w_up", [1, D, DFF])
    ffn_d = dram("ffn_w_down", [1, DFF, D])
    router_w = dram("router_w", [1, D, 8])
    if 1 in layers:
        moe_g = dram("moe_w_gate", [1, NEXP, D, DFE])
        moe_u = dram("moe_w_up", [1, NEXP, D, DFE])
        moe_d = dram("moe_w_down", [1, NEXP, DFE, D])
    outT_d = dram("outT", [D, T], kind="ExternalOutput")
    scrM = nc.dram_tensor("scrM", [4, TB], F32).ap()
    scrS = nc.dram_tensor("scrS", [1, 16], F32).ap()
    dump_d = None
    if dump is not None:
        dump_d = dram("dump", dump, kind="ExternalOutput")

    def sb(name, shape, dt=F32):
        return st.enter_context(nc.sbuf_tensor(name, list(shape), dt))

    XT = sb("XT", [128, 8, T])
    CON = sb("CON", [128, NCONST])
    COLS = sb("COLS", [128, NCOLS])
    IDB = sb("IDB", [128, 128], BF16)
    ONB = sb("ONB", [128, 128], BF16)
    CACT = sb("CACT", [128, 8], BF16)
    CTF = sb("CTF", [128, 8])
    GB = sb("GB", [4, 4])
    NFB = sb("NFB", [4, 2])
    RB = sb("RB", [128, 8])
    MODC = sb("MODC", [128, 48])
    MODC2 = sb("MODC2", [128, 48])
    MODS = [MODC, MODC2]
    CUR = {"mod": MODC}
    A1 = sb("A1", [128, 8])
    A2 = sb("A2", [128, 8])
    CWH = sb("CWH", [128, 64])
    HGH = sb("HGH", [128, 8])
    G1H = sb("G1H", [128, 8])
    CST = sb("CST", [128, 4, 2, 260])
    CBF = sb("CBF", [128, 4, 2, 260], BF16)
    QTAIL = sb("QTAIL", [128, 16, 4], BF16)
    UTAIL = sb("UTAIL", [128, 4, 16])
    MCAR = sb("MCAR", [4, 1])
    WIF = sb("WIF", [128, 8, 8], BF16)
    PWB = sb("PWB", [128, 4, 128], BF16)
    WS = sb("WS", [128, 6, 4096], BF16)
    BIG = sb("BIG", [128, 72 * 256])

    ident = CON[:, C_ID:C_ID + 128]
    maskbig = CON[:, C_MASK:C_MASK + 128]

    class Arena:
        def __init__(self):
            self.off = 0

        def f32(self, n_elems, shape=None, parts=128):
            a = self.off
            self.off += n_elems
            v = BIG[0:parts, a:a + n_elems]
            return v

        def bf(self, n_elems):
            w = (n_elems + 1) // 2
            a = self.off
            self.off += w
            return BIG[:, a:a + w].bitcast(BF16)[:, 0:n_elems]

    PSF = [st.enter_context(nc.psum_tensor("psf%d" % i, [128, 512], F32)) for i in range(6)]
    PSB = [st.enter_context(nc.psum_tensor("psb%d" % i, [128, 1024], BF16)) for i in range(2)]
    psctr = [0]

    def psum():
        i = psctr[0] % 6
        psctr[0] += 1
        return PSF[i], ("ps", i)

    def mm(out_ap, pairs, reads, writes):
        pairs = list(pairs)

        def fn(e, out_ap=out_ap, pairs=pairs):
            n = len(pairs)
            ins = None
            for i, (l, r) in enumerate(pairs):
                ins = e.matmul(out_ap, lhsT=l, rhs=r, start=(i == 0), stop=(i == n - 1))
            return ins
        P.op(PE, fn, reads, writes)

    def tr(out_ap, in_ap, idn, reads, writes):
        P.op(PE, lambda e, o=out_ap, i=in_ap, d=idn: e.transpose(o, i, d), reads, writes)

    def act(out_ap, in_ap, func, reads, writes, bias=0.0, scale=1.0, accum=None):
        def fn(e, o=out_ap, i=in_ap, f=func, b=bias, s=scale, a=accum):
            if a is None:
                return e.activation(out=o, in_=i, func=f, bias=b, scale=s)
            return e.activation(out=o, in_=i, func=f, bias=b, scale=s, accum_out=a)
        P.op(ACT, fn, reads, writes)

    def tt(eng, out_ap, a, b, op, reads, writes):
        P.op(eng, lambda e, o=out_ap, a=a, b=b, op=op: e.tensor_tensor(out=o, in0=a, in1=b, op=op),
             reads, writes)

    def ts(eng, out_ap, a, s1, op0, reads, writes, s2=None, op1=None):
        def fn(e, o=out_ap, a=a, s1=s1, s2=s2, op0=op0, op1=op1):
            if op1 is None:
                return e.tensor_scalar(out=o, in0=a, scalar1=s1, scalar2=None, op0=op0)
            return e.tensor_scalar(out=o, in0=a, scalar1=s1, scalar2=s2, op0=op0, op1=op1)
        P.op(eng, fn, reads, writes)

    def stt(out_ap, a, s, b, op0, op1, reads, writes):
        P.op(DVE, lambda e, o=out_ap, a=a, s=s, b=b, op0=op0, op1=op1:
             e.scalar_tensor_tensor(out=o, in0=a, scalar=s, in1=b, op0=op0, op1=op1), reads, writes)

    def cp(eng, out_ap, in_ap, reads, writes):
        if eng == ACT:
            P.op(ACT, lambda e, o=out_ap, i=in_ap: e.copy(out=o, in_=i), reads, writes)
        else:
            P.op(eng, lambda e, o=out_ap, i=in_ap: e.tensor_copy(out=o, in_=i), reads, writes)

    def scan(out_ap, d0, d1, init, op0, op1, reads, writes):
        P.op(DVE, lambda e, o=out_ap, d0=d0, d1=d1, i=init, op0=op0, op1=op1:
             e.tensor_tensor_scan(out=o, data0=d0, data1=d1, initial=i, op0=op0, op1=op1),
             reads, writes)

    def memset(eng, ap, val, writes):
        P.op(eng, lambda e, a=ap, v=val: e.memset(a, v), (), writes)

    ALLK = []

    def barrier():
        n = P.nbar
        P.nbar += 1
        for e in (PE, ACT, DVE, POOL):
            pass
        keys = list(P.last_w.keys())
        rkeys = list(P.readers.keys())
        allk = list(set(keys) | set(rkeys))
        P.op(DVE, lambda e: e.memset(BARS[:, 0:1], 0.0), (), allk)
        for e in (ACT, POOL, PE, SP):
            P.op(e, lambda x: x.nop(nofuse=True), allk, ())

    BARS = sb("BARS", [128, 4])

    wslot_ctr = [0]

    def wslot(n=1):
        nslots = 6
        i = wslot_ctr[0]
        if (i % nslots) + n > nslots:
            i += nslots - (i % nslots)
        wslot_ctr[0] = i + n
        return [(i + j) % nslots for j in range(n)]

    def wview(slots, shape_str, **kw):
        s0 = slots[0]
        n = len(slots)
        v = WS[:, s0:s0 + n, :].rearrange("p s n -> p (s n)")
        return v.rearrange(shape_str, **kw)

    def wkeys(slots):
        return [("ws", s) for s in slots]

    def wdma(dst_ap, src_ap, slots):
        P.dma(POOL, dst_ap, src_ap, writes=wkeys(slots), chan=("ws", slots[0]))

    P.dma(SP, CON[:], consts_d, writes=["con"], chan="con")
    P.dma(SP, COLS[:], cols_d, writes=["cols"], chan="cols")
    P.dma(SP, CTF[:], cT_d, writes=["ctf"], chan="misc")
    P.dma(SP, GB[:], gb_d, writes=["gb"], chan="misc2")
    P.dma(SP, RB[:], rb_d, writes=["rb"], chan="misc3")
    for kc in range(8):
        P.dma(SP, XT[:, kc, :], xT_d[kc * 128:(kc + 1) * 128, :], writes=[("x", kc, p) for p in range(NP)],
              chan=("xload", kc))
    cp(DVE, IDB[:], ident, ["con"], ["idb"])
    memset(DVE, ONB[:], 1.0, ["onb"])
    act(CACT[:], CTF[:], AF.Silu, ["ctf"], ["cact"])
    ts(DVE, NFB[:, 0:1], GB[:, 1:2], -1.0, ALU.mult, ["gb"], ["nfb"])
    ts(DVE, NFB[:, 1:2], GB[:, 3:4], -1.0, ALU.mult, ["gb"], ["nfb"])

    ddump = {"n": 0}
    NTMP = {}

    WQ = {"reqs": [], "idx": {}, "issued": 0, "views": {}, "live": {}, "cur": 0}

    def wq_add(key, fn, nsl):
        WQ["idx"][key] = len(WQ["reqs"])
        WQ["reqs"].append((key, fn, nsl))

    def peek_wslot(n):
        nslots = 6
        i = wslot_ctr[0]
        if (i % nslots) + n > nslots:
            i += nslots - (i % nslots)
        return [(i + j) % nslots for j in range(n)]

    def wq_issue_upto(i, strict):
        i = min(i, len(WQ["reqs"]) - 1)
        while WQ["issued"] <= i:
            key, fn, nsl = WQ["reqs"][WQ["issued"]]
            slots = peek_wslot(nsl)
            busy = set()
            for v in WQ["live"].values():
                busy.update(v)
            if any(s_ in busy for s_ in slots):
                assert not strict, ("weight slot conflict", key, slots, WQ["live"])
                return
            WQ["views"][key] = fn()
            WQ["live"][key] = slots
            WQ["issued"] += 1

    def need(key):
        i = WQ["idx"][key]
        wq_issue_upto(i, True)
        WQ["cur"] = max(WQ["cur"], i)
        return WQ["views"][key]

    def prefetch(n):
        wq_issue_upto(WQ["cur"] + n, False)

    def release(*keys):
        for k in keys:
            WQ["live"].pop(k, None)

    def rmsnorm_mod(l, src_keys_fn, p, Acol, shcol_base, dst_fn, dst_key, ar, tagp):
        sq = [ar.bf(TB) for _ in range(2)]
        rr = ar.f32(TB)
        t0_ = ar.off
        tmp = [ar.f32(TB) for _ in range(2)]
        NTMP["rr"] = rr
        NTMP["sq0"] = sq[0]
        NTMP["tmp01"] = BIG[:, t0_:t0_ + 2 * TB]
        pt, pk = psum()
        for kc in range(8):
            act(sq[kc % 2], XT[:, kc, p * TB:(p + 1) * TB], AF.Square, [("x", kc, p)], [(tagp, "sq", kc % 2)])
            P.op(PE, lambda e, kc=kc, pt=pt, s=sq[kc % 2]: e.matmul(pt[:], lhsT=ONB[:], rhs=s,
                                                                     start=(kc == 0), stop=(kc == 7)),
                 [(tagp, "sq", kc % 2), "onb"], [pk])
        ts(DVE, rr, pt[:], 1024.0 * EPS, ALU.add, [pk], [(tagp, "rr")])
        act(rr, rr, AF.Ln, [(tagp, "rr")], [(tagp, "rr")])
        act(rr, rr, AF.Exp, [(tagp, "rr")], [(tagp, "rr")], scale=-0.5)
        for kc in range(8):
            stt(tmp[kc % 2], XT[:, kc, p * TB:(p + 1) * TB], Acol[:, kc:kc + 1], rr, ALU.mult, ALU.mult,
                [("x", kc, p), (tagp, "rr"), "modc"], [(tagp, "tmp", kc % 2)])
            act(dst_fn(kc), tmp[kc % 2], AF.Identity, [(tagp, "tmp", kc % 2), "modc"], [dst_key],
                bias=CUR["mod"][:, shcol_base + kc:shcol_base + kc + 1])

    def ada_blocks_now(l, blks):
        MOD = MODS[l]
        c0 = l * LC
        pt, pk = psum()
        for blk in blks:
            sl = wslot(1)
            wv = wview(sl, "p (k n) -> p k n", k=8)
            wdma(wv, w_ada[l][:, blk * 512:(blk + 1) * 512].rearrange("(kc p) n -> p kc n", p=128), sl)
            for jj in range(4):
                j = blk * 4 + jj
                mm(pt[:, j:j + 1], [(wv[:, kc, jj * 128:(jj + 1) * 128], CACT[:, kc:kc + 1]) for kc in range(8)],
                   wkeys(sl) + ["cact"], [pk])
        lo, hi = 4 * min(blks), 4 * max(blks) + 4
        tt(DVE, MOD[:, lo:hi], pt[:, lo:hi], COLS[:, c0 + 16 + lo:c0 + 16 + hi], ALU.add, [pk, "cols"], ["modc"])

    def derive_early(l):
        MOD = MODS[l]
        c0 = l * LC
        ts(DVE, A1[:], MOD[:, 8:16], 1.0, ALU.add, ["modc", ("mod", l)], ["a1"], s2=32.0, op1=ALU.mult)
        tt(DVE, A1[:], A1[:], COLS[:, c0:c0 + 8], ALU.mult, ["a1", "cols"], ["modc", ("mod", l)])
        ts(DVE, CWH[:], COLS[:, c0 + 64:c0 + 128], 0.5, ALU.mult, ["cols"], ["cwh"])
        ts(DVE, HGH[:], COLS[:, c0 + 132:c0 + 140], 0.5, ALU.mult, ["cols"], ["hgh"])

    def derive_late(l):
        MOD = MODS[l]
        c0 = l * LC
        ts(DVE, G1H[:], MOD[:, 16:24], 0.5, ALU.mult, ["modc", ("mod", l)], ["g1h"])
        ts(DVE, A2[:], MOD[:, 32:40], 1.0, ALU.add, ["modc", ("mod", l)], ["a2"], s2=32.0, op1=ALU.mult)
        tt(DVE, A2[:], A2[:], COLS[:, c0 + 8:c0 + 16], ALU.mult, ["a2", "cols"], ["modc", ("mod", l)])

    def ada_block_deferred(lsrc, blk, psum_fn):
        MOD = MODS[lsrc]
        c0s = lsrc * LC
        sl, wv = need(("ada", lsrc, blk))
        pt, pk = psum_fn()
        for jj in range(4):
            mm(pt[:, jj:jj + 1], [(wv[:, kc, jj * 128:(jj + 1) * 128], CACT[:, kc:kc + 1]) for kc in range(8)],
               wkeys(sl) + ["cact"], [pk])
        tt(DVE, MOD[:, 4 * blk:4 * blk + 4], pt[:, 0:4], COLS[:, c0s + 16 + 4 * blk:c0s + 16 + 4 * blk + 4], ALU.add,
           [pk, "cols"], [("mod", lsrc)])
        release(("ada", lsrc, blk))

    def mixer_layer(l):
        c0 = l * LC
        P.dma(POOL, WIF[:], w_in[l][:, 4096:4104].rearrange("(kc p) n -> p kc n", p=128), writes=["wif"], chan="wif")
        P.dma(POOL, PWB[:], pool_w[l].rearrange("g c d -> c g d"), writes=["pwb"], chan="pwb")
        memset(DVE, CST[:], 0.0, ["cst"])
        memset(DVE, CBF[:], 0.0, [("cbf", h_) for h_ in range(4)])
        memset(DVE, QTAIL[:], 0.0, ["qtail"])
        memset(DVE, UTAIL[:], 0.0, ["utail"])
        memset(DVE, MCAR[:], 0.0, ["mcar"])
        cbf_par = [0]

        def req_head(h):
            def fn():
                sl = wslot(2)
                whd = wview(sl, "p (k j c) -> p k j c", k=8, j=4)
                for j4 in range(4):
                    wdma(whd[:, :, j4, :], w_in[l][:, j4 * 1024 + h * 256:j4 * 1024 + (h + 1) * 256].rearrange("(kc p) c -> p kc c", p=128), sl)
                return sl, whd
            return fn

        def req_cols(src, c_lo, ncols, kcn=8):
            def fn():
                sl = wslot(1)
                wv = wview(sl, "p (k n) -> p k n", k=8)
                wdma(wv[:, 0:kcn, 0:ncols], src[:, c_lo:c_lo + ncols].rearrange("(kc p) n -> p kc n", p=128), sl)
                return sl, wv
            return fn

        def req_wout():
            sl = wslot(2)
            wv = wview(sl, "p (k n) -> p k n", k=8)
            wdma(wv, w_out[l].rearrange("(kc p) n -> p kc n", p=128), sl)
            return sl, wv

        ADA = {}
        if l == layers[0]:
            for i_, blk in enumerate(range(4, 12)):
                ADA.setdefault((0, i_ // 2), []).append((l, blk))
            if len(layers) > 1:
                for blk in range(12):
                    ADA.setdefault((1 + blk // 4, blk % 4), []).append((layers[1], blk))
        for p in range(NP):
            for h in range(4):
                wq_add(("head", p, h), req_head(h), 2)
                for (ls_, blk_) in ADA.get((p, h), []):
                    wq_add(("ada", ls_, blk_), req_cols(w_ada[ls_], blk_ * 512, 512), 1)
            wq_add(("u", p), req_cols(w_in[l], 4104, 512), 1)
            for grp in range(2):
                wq_add(("ga", p, grp), req_cols(w_in[l], 4616 + grp * 512, 512), 1)
                wq_add(("pa", p, grp), req_cols(proj_a[l], grp * 512, 512), 1)
            for grp in range(2):
                wq_add(("gb", p, grp), req_cols(w_in[l], 5640 + grp * 512, 512), 1)
                wq_add(("pb", p, grp), req_cols(proj_b[l], grp * 512, 512, kcn=4), 1)
            wq_add(("wo", p), req_wout, 2)

        for p in range(NP):
            tsl = slice(p * TB, (p + 1) * TB)
            if p == 0:
                barrier()
            ar = Arena()
            HP = ar.bf(8 * TB).rearrange("p (k t) -> p k t", k=8)
            HAT = ar.bf(8 * TB).rearrange("p (k t) -> p k t", k=8)
            HBT = ar.bf(4 * TB).rearrange("p (k t) -> p k t", k=4)
            rmsnorm_mod(l, None, p, A1, 0, lambda kc: HP[:, kc, :], "hp", ar, "n1")
            if p > 0:
                barrier()
            chk(3)
            SM = ar.f32(64, parts=4)
            nbt, gmx, mloc, btot, mnext, mprev, d1, sold = [SM[:, i * 4:(i + 1) * 4] for i in range(8)]
            GCOL = ar.f32(64)
            SOLDB = ar.f32(16)
            MB = ar.f32(TB)
            HEADMARK = ar.off
            R32 = [ar.f32(TB, parts=32) for _ in range(5)]
            R = [r_[0:4, :] for r_ in R32]
            pi, pik = psum()
            pf, pfk = psum()
            mm(pi[0:4, :], [(WIF[:, kc, 0:4], HP[:, kc, :]) for kc in range(8)], ["wif", "hp"], [pik])
            mm(pf[0:4, :], [(WIF[:, kc, 4:8], HP[:, kc, :]) for kc in range(8)], ["wif", "hp"], [pfk])
            rA, rB, rC, rD, rE = R
            ts(DVE, rA, pi[0:4, :], GB[:, 2 * l:2 * l + 1], ALU.add, [pik, "gb"], ["rA"])
            act(rB, pf[0:4, :], AF.Exp, [pfk, "nfb"], ["rB"], bias=NFB[:, l:l + 1], scale=-1.0)
            act(rB, rB, AF.Ln, ["rB"], ["rB"], bias=1.0)
            chk(3.1)
            scan(rC, CON[0:4, C_RMUL:C_RMUL + TB], rB, 0.0, ALU.mult, ALU.add, ["rB", "con"], ["rC"])
            tt(DVE, rA, rA, rC, ALU.add, ["rA", "rC"], ["rA"])
            scan(rD, CON[0:4, C_RADD:C_RADD + TB], rA, 0.0, ALU.add, ALU.max, ["rA", "con"], ["rD"])
            chk(3.2)
            nbt1, gmx1, mloc1, btot1, mnext1, mprev1, d11, sold1 = [x_[:, 0:1] for x_ in (nbt, gmx, mloc, btot, mnext, mprev, d1, sold)]
            cp(DVE, nbt1, rC[:, TB - 1:TB], ["rC"], ["sm"])
            cp(DVE, gmx1, rD[:, TB - 1:TB], ["rD"], ["sm"])
            tt(DVE, mloc1, gmx1, nbt1, ALU.subtract, ["sm"], ["sm"])
            ts(DVE, btot1, nbt1, -1.0, ALU.mult, ["sm"], ["sm"])
            tt(DVE, mnext1, btot1, MCAR[:, 0:1], ALU.add, ["sm", "mcar"], ["sm"])
            tt(DVE, mnext1, mnext1, mloc1, ALU.max, ["sm"], ["sm"])
            cp(DVE, mprev1, MCAR[:, 0:1], ["mcar", "sm"], ["sm"])
            cp(DVE, MCAR[:, 0:1], mnext1, ["sm"], ["mcar"])
            tt(DVE, d11, btot1, mnext1, ALU.subtract, ["sm"], ["sm"])
            tt(DVE, sold1, d11, mprev1, ALU.add, ["sm"], ["sm"])
            act(sold1, sold1, AF.Exp, ["sm"], ["sm"])
            chk(3.3)
            ts(DVE, rD, rD, mprev1, ALU.max, ["rD", "sm"], ["rD"])
            ts(DVE, rB, rD, mprev1, ALU.subtract, ["rD", "sm", "rB"], ["rB"])
            act(rB, rB, AF.Exp, ["rB"], ["rB"], bias=float(-np.log(16.0)), scale=-1.0)
            tt(DVE, rC, rC, rD, ALU.subtract, ["rC", "rD"], ["rC"])
            act(rC, rC, AF.Exp, ["rC"], ["rC"])
            ts(DVE, rE, rA, d11, ALU.add, ["rA", "sm"], ["rE"])
            act(rE, rE, AF.Exp, ["rE"], ["rE"])
            chk(3.4)
            pg, pgk = psum()
            for qi, (rr_, rk) in enumerate([(R32[0], "rA"), (R32[4], "rE"), (R32[2], "rC"), (R32[1], "rB")]):
                for c in range(NCH):
                    a_ = qi * 4 + c
                    tr(pg[:, a_ * 32:(a_ + 1) * 32], rr_[:, c * 128:(c + 1) * 128], ident[0:32, 0:32], [rk, "con"], [pgk])
            cp(DVE, GCOL.rearrange("p (a h) -> p a h", h=4), pg[:].rearrange("p (a b) -> p a b", b=32)[:, :, 0:4],
               [pgk], ["gcol"])
            chk(3.5)
            P.dma(SP, scrS[:, 0:4].rearrange("o (h c) -> (o h) c", h=4), sold1, reads=["sm"], writes=["scrS"], chan="scrS")
            P.dma(SP, SOLDB[:, 0:4], scrS[:, 0:4].partition_broadcast(128), reads=["scrS"], writes=["soldb"], chan="soldb")
            P.dma(SP, scrM, rD, reads=["rD"], writes=["scrM"], chan="scrM")
            barrier()
            ar.off = HEADMARK

            chk(4)
            RAWB = ar.bf(TB + 4)
            T5 = ar.f32(TB)
            DG = [ar.bf(128) for _ in range(4)]
            HB = []
            for par_ in range(2):
                HB.append(dict(
                    QT=ar.bf(2 * TB).rearrange("p (k t) -> p k t", k=2),
                    KT=ar.bf(2 * TB).rearrange("p (k t) -> p k t", k=2),
                    KTM=ar.bf(NCH * 256).rearrange("p (c f) -> p c f", c=NCH),
                    VTM=ar.bf(NCH * 256).rearrange("p (c f) -> p c f", c=NCH),
                    OG=ar.bf(NCH * 256).rearrange("p (c f) -> p c f", c=NCH),
                    k=par_))
            DT4 = ar.f32(1280)
            PTX = ar.bf(1280 - 512)
            VW4 = ar.bf(NCH * 256).rearrange("p (c f) -> p c f", c=NCH)
            WHB = ar.bf(4)
            ND = ar.f32(NCH * 256).rearrange("p (c f) -> p c f", c=NCH)
            JUNK = ar.bf(256)
            HATM = ar.bf(NCH * 256).rearrange("p (c f) -> p c f", c=NCH)
            SMALL = ar.f32(64)
            SIGT = [ar.f32(256) for _ in range(2)]
            TMPD4 = NTMP["rr"]
            PT4 = NTMP["sq0"]
            ND1 = NTMP["tmp01"].rearrange("p (c f) -> p c f", c=NCH)
            K_RR, K_SQ0, K_T0, K_T1 = ("n1", "rr"), ("n1", "sq", 0), ("n1", "tmp", 0), ("n1", "tmp", 1)
            pj_ctr = [0]

            def psum_pj():
                i = 4 + (pj_ctr[0] % 2)
                pj_ctr[0] += 1
                return PSF[i], ("ps", i)

            def c3(a, n):
                return a.rearrange("p (c t) -> p c t", c=NCH)

            def proj_gen(h, B):
                QT, KT, KTM, VTM, OG = B["QT"], B["KT"], B["KTM"], B["VTM"], B["OG"]
                bq = B["k"]
                kq, kk, kkt, kv, ko = ("qt", bq), ("kt", bq), ("ktm", bq), ("vtm", bq), ("og", bq)
                sl, whd = need(("head", p, h))
                prefetch(2)
                for qk, dstT, dkey in ((0, QT, kq), (1, KT, kk)):
                    for fc in range(2):
                        j = qk * 8 + h * 2 + fc
                        pt, pk = psum_pj()
                        mm(pt[:], [(whd[:, kc, qk, fc * 128:(fc + 1) * 128], HP[:, kc, :]) for kc in range(8)],
                           wkeys(sl) + ["hp"], [pk])
                        cp(DVE, RAWB[:, 0:3], QTAIL[:, j, 0:3], ["qtail"], ["rawb"])
                        cp(ACT, RAWB[:, 3:3 + TB], pt[:], [pk], ["rawb"])
                        cw = CWH[:, j * 4:j * 4 + 4]
                        for tap in range(4):
                            ts(DVE, DG[tap], ident, cw[:, tap:tap + 1], ALU.mult, ["con", "cwh"], [("dg", tap)])
                        yield
                        py, pyk = psum_pj()
                        mm(py[:], [(DG[tap], RAWB[:, tap:tap + TB]) for tap in range(4)],
                           [("dg", t_) for t_ in range(4)] + ["rawb"], [pyk])
                        cp(DVE, QTAIL[:, j, 0:3], RAWB[:, TB:TB + 3], ["rawb"], ["qtail"])
                        yield
                        act(T5, py[:], AF.Tanh, [pyk], ["t5"])
                        yield
                        stt(dstT[:, fc, :], T5, 1.0, py[:], ALU.add, ALU.mult, ["t5", pyk], [dkey])
                        yield
                for fc in range(2):
                    for c in range(NCH):
                        tr(PSB[0][:, c * 256 + fc * 128:c * 256 + (fc + 1) * 128], KT[:, fc, c * 128:(c + 1) * 128],
                           IDB[:], [kk, "idb"], ["psb0"])
                cp(ACT, KTM.rearrange("p c f -> p (c f)"), PSB[0][:, 0:1024], ["psb0"], [kkt])
                yield
                for c in range(NCH):
                    pt, pk = psum_pj()
                    mm(pt[:], [(HP[:, kc, c * 128:(c + 1) * 128], whd[:, kc, 2:4, :].rearrange("p j c -> p (j c)"))
                               for kc in range(8)], wkeys(sl) + ["hp"], [pk])
                    cp(DVE, VTM[:, c, :], pt[:, 0:256], [pk], [kv])
                    act(SIGT[c % 2], pt[:, 256:512], AF.Tanh, [pk], [("sigt", c % 2)], scale=0.5)
                    ts(DVE, OG[:, c, :], SIGT[c % 2], 1.0, ALU.add, [("sigt", c % 2)], [ko])
                    yield
                release(("head", p, h))
                for (ls_, blk_) in ADA.get((p, h), []):
                    ada_block_deferred(ls_, blk_, psum_pj)
                    yield
                prefetch(3)

            def chunk_gen(h, B):
                QT, KT, KTM, VTM, OG = B["QT"], B["KT"], B["KTM"], B["VTM"], B["OG"]
                bq = B["k"]
                kq, kk, kkt, kv, ko = ("qt", bq), ("kt", bq), ("ktm", bq), ("vtm", bq), ("og", bq)
                P.dma(SP, MB, scrM[h:h + 1, :].partition_broadcast(128), reads=["scrM"], writes=["mb"], chan="mb")
                wh4 = GCOL[:, 16 + h:32:4]
                df4 = GCOL[:, 32 + h:48:4]
                wi4 = GCOL[:, 48 + h:64:4]
                bk = lambda i: ("ps", i)
                WJ = [512, 384, 256, 128]
                SREG = [(0, 0), (1, 0), (3, 128), (1, 384)]
                DOFF = [0, 512, 896, 1152]
                for j in range(NCH):
                    bnk, co = SREG[j]
                    mm(PSF[bnk][:, co:co + WJ[j]],
                       [(KT[:, kc, j * 128:(j + 1) * 128], QT[:, kc, j * 128:TB]) for kc in range(2)], [kk, kq], [bk(bnk)])
                tt(DVE, VW4, VTM, wh4.unsqueeze(2).to_broadcast([128, NCH, 256]), ALU.mult, [kv, "gcol"], ["vw"])
                cp(DVE, WHB, wh4, ["gcol"], ["whb"])
                yield
                for kc in range(2):
                    mm(PSF[2][:, kc * 256:(kc + 1) * 256],
                       [(KTM[:, c, kc * 128:(kc + 1) * 128], VW4[:, c, :]) for c in range(NCH)], [kkt, "vw"], [bk(2)])
                for kc in range(2):
                    mm(PSF[3][:, 8 + kc:9 + kc],
                       [(KTM[:, c, kc * 128:(kc + 1) * 128], WHB[:, c:c + 1]) for c in range(NCH)], [kkt, "whb"], [bk(3)])
                tt(DVE, c3(TMPD4, 4), c3(MB, 4), maskbig.unsqueeze(1).to_broadcast([128, NCH, 128]), ALU.add,
                   ["mb", "con", K_RR], [K_RR, "tmpd"])
                yield
                for j in range(NCH):
                    gcol = GCOL[:, j * 4 + h:j * 4 + h + 1]
                    act(DT4[:, DOFF[j]:DOFF[j] + 128], TMPD4[:, j * 128:(j + 1) * 128], AF.Exp, ["tmpd", "gcol"], ["dt"],
                        bias=gcol, scale=-1.0)
                    if j < 3:
                        act(DT4[:, DOFF[j] + 128:DOFF[j] + WJ[j]], MB[:, (j + 1) * 128:TB], AF.Exp, ["mb", "gcol"], ["dt"],
                            bias=gcol, scale=-1.0)
                    yield
                PT = [PT4[:, 0:512], PTX[:, 0:384], PTX[:, 384:640], PTX[:, 640:768]]
                for j in range(NCH):
                    bnk, co = SREG[j]
                    stt(PT[j], PSF[bnk][:, co:co + WJ[j]], 1.0 / 16.0, DT4[:, DOFF[j]:DOFF[j] + WJ[j]], ALU.mult, ALU.mult,
                        [bk(bnk), "dt", K_SQ0], [K_SQ0, "pt"])
                yield
                sb_ = SOLDB[:, h:h + 1]
                stt(CST[:, h, :, 0:256], CST[:, h, :, 0:256], sb_, PSF[2][:].rearrange("p (k f) -> p k f", k=2),
                    ALU.mult, ALU.add, ["cst", "soldb", bk(2)], ["cst"])
                stt(CST[:, h, :, 256:257], CST[:, h, :, 256:257], sb_,
                    PSF[3][:, 8:10].rearrange("p (k f) -> p k f", k=2),
                    ALU.mult, ALU.add, ["cst", "soldb", bk(3)], ["cst"])
                yield
                for c in range(NCH):
                    bnk = 0 if c < 2 else 1
                    mm(PSF[bnk][:, (c % 2) * 256:(c % 2 + 1) * 256],
                       [(PT[j][:, (c - j) * 128:(c - j + 1) * 128], VTM[:, j, :]) for j in range(c + 1)], ["pt", kv], [bk(bnk)])
                for c in range(NCH):
                    mm(PSF[3][:, c:c + 1],
                       [(PT[j][:, (c - j) * 128:(c - j + 1) * 128], ONB[:, 0:1]) for j in range(c + 1)], ["pt", "onb"], [bk(3)])
                cp(ACT, ND1[:, 0:2, :].rearrange("p c f -> p (c f)"), PSF[0][:], [bk(0), K_T0, K_T1], [K_T0, K_T1, "nd1"])
                cp(ACT, ND1[:, 2:4, :].rearrange("p c f -> p (c f)"), PSF[1][:], [bk(1), K_T0, K_T1], [K_T0, K_T1, "nd1"])
                yield
                for c in range(NCH):
                    csl = slice(c * 128, (c + 1) * 128)
                    bnk = 0 if c < 2 else 1
                    mm(PSF[bnk][:, (c % 2) * 256:(c % 2 + 1) * 256],
                       [(QT[:, kc, csl], CBF[:, h, kc, 0:256]) for kc in range(2)], [kq, ("cbf", h)], [bk(bnk)])
                for c in range(NCH):
                    csl = slice(c * 128, (c + 1) * 128)
                    mm(PSF[3][:, 4 + c:5 + c], [(QT[:, kc, csl], CBF[:, h, kc, 256:257]) for kc in range(2)],
                       [kq, ("cbf", h)], [bk(3)])
                yield
                for c in range(NCH):
                    bnk = 0 if c < 2 else 1
                    stt(ND[:, c, :], PSF[bnk][:, (c % 2) * 256:(c % 2 + 1) * 256], GCOL[:, 48 + c * 4 + h:48 + c * 4 + h + 1],
                        ND1[:, c, :], ALU.mult, ALU.add, [bk(bnk), "nd1", "gcol", K_T0, K_T1], ["nd"])
                    if c % 2 == 1:
                        yield
                cp(ACT, CBF[:, h, :, 0:257], CST[:, h, :, 0:257], ["cst"], [("cbf", h)])
                DEN8, DEN, dd, r1, t1, u1, scl, ssq = [SMALL[:, i * 8:i * 8 + 4] for i in range(8)]
                cp(DVE, SMALL[:, 0:8], PSF[3][:, 0:8], [bk(3)], ["small"])
                tt(DVE, DEN, SMALL[:, 4:8], wi4, ALU.mult, ["small", "gcol"], ["small"])
                tt(DVE, DEN, DEN, SMALL[:, 0:4], ALU.add, ["small"], ["small"])
                for c in range(NCH):
                    act(JUNK, ND[:, c, :], AF.Square, ["nd"], ["junk", "small_ssq"], accum=ssq[:, c:c + 1])
                yield
                ts(DVE, dd, DEN, -1.0, ALU.mult, ["small"], ["small"])
                tt(DVE, dd, dd, DEN, ALU.max, ["small"], ["small"])
                yield
                tt(DVE, dd, dd, df4, ALU.max, ["gcol", "small"], ["small"])
                P.op(DVE, lambda e, o=r1, i=dd: e.reciprocal(out=o, in_=i), ["small"], ["small"])
                yield
                tt(DVE, t1, ssq, r1, ALU.mult, ["small", "small_ssq"], ["small"])
                tt(DVE, t1, t1, r1, ALU.mult, ["small"], ["small"])
                yield
                ts(DVE, u1, t1, 1.0 / 256.0, ALU.mult, ["small"], ["small"], s2=EPS, op1=ALU.add)
                act(u1, u1, AF.Ln, ["small"], ["small_u"])
                yield
                act(u1, u1, AF.Exp, ["small_u"], ["small_u"], scale=-0.5)
                tt(DVE, scl, u1, r1, ALU.mult, ["small", "small_u"], ["small"])
                yield
                for c in range(NCH):
                    stt(HATM[:, c, :], ND[:, c, :], scl[:, c:c + 1], OG[:, c, :], ALU.mult, ALU.mult,
                        ["nd", "small", ko], ["hatm"])
                yield
                for c in range(NCH):
                    for fc in range(2):
                        tr(PSB[1][:, fc * 512 + c * 128:fc * 512 + (c + 1) * 128], HATM[:, c, fc * 128:(fc + 1) * 128],
                           IDB[:], ["hatm", "idb"], ["psb1"])
                for fc in range(2):
                    kcg = 2 * h + fc
                    act(HAT[:, kcg, :], PSB[1][:, fc * 512:(fc + 1) * 512], AF.Identity, ["psb1", "hgh"], ["hat"],
                        scale=HGH[:, kcg:kcg + 1])

            def run_interleaved(gens):
                gens = list(gens)
                while gens:
                    for g_ in list(gens):
                        try:
                            next(g_)
                        except StopIteration:
                            gens.remove(g_)

            run_interleaved([proj_gen(0, HB[0])])
            for h in range(4):
                gl = [chunk_gen(h, HB[h % 2])]
                if h < 3:
                    gl.append(proj_gen(h + 1, HB[(h + 1) % 2]))
                run_interleaved(gl)
            if l == layers[0] and p == 0:
                derive_late(l)

            chk(5)
            barrier()
            ar.off = HEADMARK
            UB = ar.f32(TB + 16)
            LV = [ar.f32(TB + 16) for _ in range(2)]
            PLD = ar.bf(TB)
            T16 = ar.f32(16)
            MG = ar.bf(8 * TB).rearrange("p (k t) -> p k t", k=8)
            SGA = ar.f32(TB)
            SGB = ar.f32(TB)
            M2 = ar.f32(TB)

            def pool_gen():
                sl, wu = need(("u", p))
                for g in range(4):
                    pt, pk = psum()
                    mm(pt[:], [(wu[:, kc, g * 128:(g + 1) * 128], HP[:, kc, :]) for kc in range(8)], wkeys(sl) + ["hp"], [pk])
                    cp(DVE, UB[:, 0:16], UTAIL[:, g, :], ["utail"], ["ub"])
                    cp(ACT, UB[:, 16:16 + TB], pt[:], [pk], ["ub"])
                    cp(DVE, UTAIL[:, g, :], UB[:, TB:TB + 16], ["ub"], ["utail"])
                    yield
                    src, skey = UB, "ub"
                    d = 1
                    for lev in range(g + 1):
                        dst = LV[lev % 2]
                        dk = ("lv", lev % 2)
                        lo = 2 * d - 1
                        tt(DVE, dst[:, lo:TB + 16], src[:, lo:TB + 16], src[:, lo - d:TB + 16 - d], ALU.add,
                           [skey], [dk])
                        src, skey = dst, dk
                        d *= 2
                        yield
                    win = float(d)
                    stt(PLD, src[:, 16:16 + TB], 1.0 / win, UB[:, 16:16 + TB], ALU.mult, ALU.subtract,
                        [skey, "ub"], ["pld"])
                    if p == 0:
                        tt(DVE, T16, src[:, 16:32], CON[:, C_INVC + g * 16:C_INVC + (g + 1) * 16], ALU.mult,
                           [skey, "con"], ["t16"])
                        tt(DVE, PLD[:, 0:16], T16, UB[:, 16:32], ALU.subtract, ["t16", "ub", "pld"], ["pld"])
                    yield
                    pt2, pk2 = psum()
                    mm(pt2[:], [(PWB[:, g, :], PLD)], ["pwb", "pld"], [pk2])
                    ts(DVE, HBT[:, g, :], pt2[:], COLS[:, c0 + 128 + g:c0 + 128 + g + 1], ALU.mult, [pk2, "cols"], ["hbt"])
                    yield
                release(("u", p))

            def merge_a_gen():
                for grp in range(2):
                    s_ga, wga = need(("ga", p, grp))
                    s_pa, wpa = need(("pa", p, grp))
                    for dd_ in range(4):
                        dc = grp * 4 + dd_
                        cs = slice(dd_ * 128, (dd_ + 1) * 128)
                        pga, pgak = psum()
                        ppa, ppak = psum()
                        mm(pga[:], [(wga[:, kc, cs], HP[:, kc, :]) for kc in range(8)], wkeys(s_ga) + ["hp"], [pgak])
                        mm(ppa[:], [(wpa[:, kc, cs], HAT[:, kc, :]) for kc in range(8)], wkeys(s_pa) + ["hat"], [ppak])
                        act(SGA, pga[:], AF.Tanh, [pgak], ["sga"], scale=0.5)
                        yield
                        stt(MG[:, dc, :], SGA, 1.0, ppa[:], ALU.add, ALU.mult, [ppak, "sga"], [("mg", dc)])
                        yield
                    release(("ga", p, grp), ("pa", p, grp))
                    prefetch(2)

            run_interleaved([pool_gen(), merge_a_gen()])
            chk(6)
            for grp in range(2):
                s_gb, wgb = need(("gb", p, grp))
                s_pb, wpb = need(("pb", p, grp))
                prefetch(2)
                for dd_ in range(4):
                    dc = grp * 4 + dd_
                    cs = slice(dd_ * 128, (dd_ + 1) * 128)
                    pgb, pgbk = psum()
                    ppb, ppbk = psum()
                    mm(pgb[:], [(wgb[:, kc, cs], HP[:, kc, :]) for kc in range(8)], wkeys(s_gb) + ["hp"], [pgbk])
                    mm(ppb[:], [(wpb[:, kc, cs], HBT[:, kc, :]) for kc in range(4)], wkeys(s_pb) + ["hbt"], [ppbk])
                    act(SGB, pgb[:], AF.Tanh, [pgbk], ["sgb"], scale=0.5)
                    stt(M2, SGB, 1.0, ppb[:], ALU.add, ALU.mult, [ppbk, "sgb"], ["m2"])
                    tt(DVE, MG[:, dc, :], MG[:, dc, :], M2, ALU.add, ["m2", ("mg", dc)], [("mg", dc)])
                release(("gb", p, grp), ("pb", p, grp))
                prefetch(2)
            s_wo, wwo = need(("wo", p))
            prefetch(2)
            for dc in range(8):
                pt, pk = psum()
                mm(pt[:], [(wwo[:, kc, dc * 128:(dc + 1) * 128], MG[:, kc, :]) for kc in range(8)],
                   wkeys(s_wo) + [("mg", k_) for k_ in range(8)], [pk])
                stt(XT[:, dc, tsl], pt[:], G1H[:, dc:dc + 1], XT[:, dc, tsl], ALU.mult, ALU.add,
                    [pk, "g1h", ("x", dc, p)], [("x", dc, p)])
            release(("wo", p))

    FIN = {"done": False, "oc": 0}

    def final_norm_pass(p, fb):
        fcol = COLS[:, 2 * LC:2 * LC + 8]
        sq, rr, OUTB = fb["sq"], fb["rr"], fb["OUTB"]
        tsl = slice(p * TB, (p + 1) * TB)
        pt, pk = psum()
        for kc in range(8):
            act(sq[kc % 2], XT[:, kc, tsl], AF.Square, [("x", kc, p)], [("fsq", kc % 2)])
            P.op(PE, lambda e, kc=kc, pt=pt, s_=sq[kc % 2]: e.matmul(pt[:], lhsT=ONB[:], rhs=s_,
                                                                      start=(kc == 0), stop=(kc == 7)),
                 [("fsq", kc % 2), "onb"], [pk])
        ts(DVE, rr, pt[:], 1024.0 * EPS, ALU.add, [pk], ["frr"])
        act(rr, rr, AF.Ln, ["frr"], ["frr"])
        act(rr, rr, AF.Exp, ["frr"], ["frr"], scale=-0.5)
        for kc in range(8):
            oc = FIN["oc"]
            ob = OUTB[oc % 4]
            obk = ("outb", oc % 4)
            FIN["oc"] = oc + 1
            stt(ob, XT[:, kc, tsl], fcol[:, kc:kc + 1], rr, ALU.mult, ALU.mult, [("x", kc, p), "frr", "cols"], [obk])
            ts(DVE, ob, ob, 32.0, ALU.mult, [obk], [obk])
            P.dma(SP, outT_d[kc * 128:(kc + 1) * 128, tsl], ob, reads=[obk], chan=obk)

    def ffn_layer(l):
        barrier()
        ar = Arena()
        H2 = ar.bf(8 * T).rearrange("p (k t) -> p k t", k=8)
        for p in range(NP):
            ar2 = Arena()
            ar2.off = 8 * T // 2
            rmsnorm_mod(l, None, p, A2, 24, lambda kc, p=p: H2[:, kc, p * TB:(p + 1) * TB], ("h2", p), ar2, "n2")
        barrier()
        ar.off = 8 * T // 2
        AT = [ar.bf(4 * TB).rearrange("p (k t) -> p k t", k=4) for _ in range(2)]
        SG = [ar.f32(TB) for _ in range(2)]
        moe = (l % 2 == 1)
        if moe:
            CB = ar.f32(T)
            LG = ar.f32(16 * 8)
            CMB = ar.f32(16 * 8)
            TMP8 = ar.f32(16 * 8)
            MX = ar.f32(32)
            RWB = ar.bf(8 * 8).rearrange("p (k n) -> p k n", k=8)
            P.dma(POOL, RWB, router_w[0].rearrange("(kc p) n -> p kc n", p=128), writes=["rwb"], chan="rwb")
            lg3 = LG.rearrange("p (t e) -> p t e", e=8)
            cm3 = CMB.rearrange("p (t e) -> p t e", e=8)
            tp3 = TMP8.rearrange("p (t e) -> p t e", e=8)
            pl, plk = psum()
            for tt_ in range(16):
                mm(pl[:, tt_ * 8:(tt_ + 1) * 8], [(H2[:, kc, tt_ * 128:(tt_ + 1) * 128], RWB[:, kc, :]) for kc in range(8)],
                   [("h2", tt_ // 4), "rwb"], [plk])
            tt(DVE, lg3, pl[:, 0:128].rearrange("p (t e) -> p t e", e=8), RB[:, 0:8].unsqueeze(1).to_broadcast([128, 16, 8]),
               ALU.add, [plk, "rb"], ["lg"])
            m1 = MX[:, 0:16]
            m2 = MX[:, 16:32]
            P.op(DVE, lambda e: e.tensor_reduce(out=m1, in_=lg3, axis=mybir.AxisListType.X, op=ALU.max), ["lg"], ["mx1"])
            tt(DVE, tp3, lg3, m1.unsqueeze(2).to_broadcast([128, 16, 8]), ALU.is_ge, ["lg", "mx1"], ["tp"])
            stt(tp3, tp3, -1e30, lg3, ALU.mult, ALU.add, ["tp", "lg"], ["tp"])
            P.op(DVE, lambda e: e.tensor_reduce(out=m2, in_=tp3, axis=mybir.AxisListType.X, op=ALU.max), ["tp"], ["mx2"])
            tt(DVE, tp3, lg3, m2.unsqueeze(2).to_broadcast([128, 16, 8]), ALU.is_ge, ["lg", "mx2", "tp"], ["tp"])
            tt(DVE, cm3, lg3, m1.unsqueeze(2).to_broadcast([128, 16, 8]), ALU.subtract, ["lg", "mx1"], ["cmb"])
            act(CMB, CMB, AF.Exp, ["cmb"], ["cmb"])
            tt(DVE, cm3, cm3, tp3, ALU.mult, ["cmb", "tp"], ["cmb"])
            tt(DVE, m2, m2, m1, ALU.subtract, ["mx1", "mx2"], ["mx2"])
            act(m2, m2, AF.Exp, ["mx2"], ["mx2"])
            ts(DVE, m2, m2, 1.0, ALU.add, ["mx2"], ["mx2"])
            P.op(DVE, lambda e: e.reciprocal(out=m2, in_=m2), ["mx2"], ["mx2"])
            tt(DVE, cm3, cm3, m2.unsqueeze(2).to_broadcast([128, 16, 8]), ALU.mult, ["cmb", "mx2"], ["cmb"])
            RID = [ar.f32(128) for _ in range(2)]
            blocks = [(e, f0, 512) for e in range(NEXP) for f0 in range(0, DFE, 512)]
            wg_d, wu_d, wd_d = moe_g[0], moe_u[0], moe_d[0]
        else:
            blocks = [(None, f0, min(512, DFF - f0)) for f0 in range(0, DFF, 512)]
            wg_d, wu_d, wd_d = ffn_g[0], ffn_u[0], ffn_d[0]
        fb = None
        if final and l == layers[-1]:
            fb = dict(sq=[ar.bf(TB) for _ in range(2)], rr=ar.f32(TB), OUTB=[ar.f32(TB) for _ in range(4)])
            FIN["done"] = True
        cur_e = [None]
        it = 0
        def issue_block(bi):
            e_, f0, F = blocks[bi]
            nfc = F // 128
            s_g, s_u, s_d = wslot(1), wslot(1), wslot(1)
            wg = wview(s_g, "p (k n) -> p k n", k=8)
            wu = wview(s_u, "p (k n) -> p k n", k=8)
            wd = wview(s_d, "p (k n) -> p k n", k=4)
            gsrc = wg_d if e_ is None else wg_d[e_]
            usrc = wu_d if e_ is None else wu_d[e_]
            dsrc = wd_d if e_ is None else wd_d[e_]
            wdma(wg[:, :, 0:F], gsrc[:, f0:f0 + F].rearrange("(kc p) n -> p kc n", p=128), s_g)
            wdma(wu[:, :, 0:F], usrc[:, f0:f0 + F].rearrange("(kc p) n -> p kc n", p=128), s_u)
            wdma(wd[:, 0:nfc, :], dsrc[f0:f0 + F, :].rearrange("(fc p) n -> p fc n", p=128), s_d)
            return (s_g, s_u, s_d, wg, wu, wd)

        wslot_ctr[0] += (-wslot_ctr[0]) % 3
        issued = {0: issue_block(0)}
        for bi, (e_, f0, F) in enumerate(blocks):
            nfc = F // 128
            s_g, s_u, s_d, wg, wu, wd = issued.pop(bi)
            if bi + 1 < len(blocks):
                issued[bi + 1] = issue_block(bi + 1)
            if moe and cur_e[0] != e_:
                cur_e[0] = e_
                for p in range(NP):
                    pc, pck = psum()
                    for i4 in range(4):
                        tt_ = p * 4 + i4
                        rid = RID[tt_ % 2]
                        ts(DVE, rid, ident, CMB[:, tt_ * 8 + e_:tt_ * 8 + e_ + 1], ALU.mult, ["cmb", "con"], [("rid", tt_ % 2)])
                        mm(pc[:, i4 * 128:(i4 + 1) * 128], [(CON[:, C_ONES:C_ONES + 128], rid)], [("rid", tt_ % 2), "con"], [pck])
                    cp(ACT, CB[:, p * TB:(p + 1) * TB], pc[:], [pck], [("cb", p)])
            for p in range(NP):
                tsl = slice(p * TB, (p + 1) * TB)
                at = AT[it % 2]
                atk = ("at", it % 2)
                it += 1
                for fc in range(nfc):
                    pgt, pgk_ = psum()
                    put, puk_ = psum()
                    fs = slice(fc * 128, (fc + 1) * 128)
                    mm(pgt[:], [(wg[:, kc, fs], H2[:, kc, tsl]) for kc in range(8)], wkeys(s_g) + [("h2", p)], [pgk_])
                    mm(put[:], [(wu[:, kc, fs], H2[:, kc, tsl]) for kc in range(8)], wkeys(s_u) + [("h2", p)], [puk_])
                    sg = SG[fc % 2]
                    sgk = ("sg", fc % 2)
                    act(sg, pgt[:], AF.Silu, [pgk_], [sgk])
                    if moe:
                        tt(POOL, sg, sg, CB[:, tsl], ALU.mult, [sgk, ("cb", p)], [sgk])
                    tt(DVE, at[:, fc, :], put[:], sg, ALU.mult, [puk_, sgk], [atk])
                for dc in range(8):
                    pt, pk = psum()
                    mm(pt[:], [(wd[:, fc, dc * 128:(dc + 1) * 128], at[:, fc, :]) for fc in range(nfc)],
                       wkeys(s_d) + [atk], [pk])
                    stt(XT[:, dc, tsl], pt[:], CUR["mod"][:, 40 + dc:40 + dc + 1], XT[:, dc, tsl], ALU.mult, ALU.add,
                        [pk, "modc", ("x", dc, p)], [("x", dc, p)])
                if fb is not None and bi == len(blocks) - 1:
                    final_norm_pass(p, fb)

    def chk(n):
        if stage <= n:
            raise _Stop()

    try:
        chk(1)
        for li_, l in enumerate(layers):
            CUR["mod"] = MODS[l]
            if li_ == 0:
                ada_blocks_now(l, [0, 1, 2, 3])
                derive_early(l)
            else:
                derive_early(l)
                derive_late(l)
            chk(2)
            mixer_layer(l)
            if ffn:
                ffn_layer(l)
    except _Stop:
        pass

    if final and FIN["done"]:
        fw = [("outb", i) for i in range(4)]
    elif final:
        barrier()
        ar = Arena()
        fb = dict(sq=[ar.bf(TB) for _ in range(2)], rr=ar.f32(TB), OUTB=[ar.f32(TB) for _ in range(4)])
        for p in range(NP):
            final_norm_pass(p, fb)
        fw = [("outb", i) for i in range(4)]
    else:
        for kc in range(8):
            P.dma(SP, outT_d[kc * 128:(kc + 1) * 128, :], XT[:, kc, :], reads=[("x", kc, p) for p in range(NP)],
                  chan=("xstore", kc))
        fw = [("xstore", kc) for kc in range(8)]
    P.emit(st, final_waits=fw)
    return st


def make_consts():
    c = np.zeros((128, NCONST), np.float32)
    c[:, C_ID:C_ID + 128] = np.eye(128, dtype=np.float32)
    s = np.arange(128)[:, None]
    t = np.arange(128)[None, :]
    c[:, C_MASK:C_MASK + 128] = np.where(s > t, np.float32(1e30), np.float32(0.0))
    c[:, C_ONES:C_ONES + 128] = 1.0
    rm = np.ones(512, np.float32)
    rm[0] = 0.0
    ra = np.zeros(512, np.float32)
    ra[0] = -1e30
    c[0:4, C_RMUL:C_RMUL + 512] = rm[None, :]
    c[0:4, C_RADD:C_RADD + 512] = ra[None, :]
    for g, win in enumerate((2, 4, 8, 16)):
        tt = np.arange(16)
        c[:, C_INVC + g * 16:C_INVC + (g + 1) * 16] = (1.0 / np.minimum(tt + 1, win)).astype(np.float32)[None, :]
    return c


def col(v):
    v = np.asarray(v, np.float32)
    return np.ascontiguousarray(v.reshape(-1, 128).T)


def make_cols(norm_mix, norm_ffn, b_ada, conv_w, pool_scale, final_norm, head_gain):
    c = np.zeros((128, NCOLS), np.float32)
    for l in range(2):
        o = l * LC
        c[:, o:o + 8] = col(norm_mix[l])
        c[:, o + 8:o + 16] = col(norm_ffn[l])
        c[:, o + 16:o + 64] = col(b_ada[l])
        cw = np.asarray(conv_w[l], np.float32)
        cc = cw.reshape(4, 16, 128).transpose(2, 1, 0).reshape(128, 64)
        c[:, o + 64:o + 128] = cc
        c[:, o + 128:o + 132] = col(pool_scale[l])
        c[:, o + 132:o + 140] = col(head_gain[l])
    c[:, 2 * LC:2 * LC + 8] = col(final_norm)
    return c


_CACHE = {}


def kernel(x, c, norm_mix, norm_ffn, w_ada, b_ada, w_in, conv_w, i_bias, f_bias, head_gain,
           pool_w, pool_scale, proj_a, proj_b, w_out, ffn_w_gate, ffn_w_up, ffn_w_down,
           router_w, router_b, moe_w_gate, moe_w_up, moe_w_down, final_norm, _layers=(0, 1), _final=True, _ffn=True, _stage=99):
    f = lambda a: np.ascontiguousarray(np.asarray(a, dtype=np.float32))
    x = f(x)
    c = f(c)
    nc = bass.Bass("TRN2", target_bir_lowering=False)
    st = build(nc, layers=_layers, final=_final, ffn=_ffn, stage=_stage)
    consts = make_consts()
    cols = make_cols(f(norm_mix), f(norm_ffn), f(b_ada), f(conv_w), f(pool_scale), f(final_norm), f(head_gain))
    gb = np.stack([f(i_bias)[0], f(f_bias)[0], f(i_bias)[1], f(f_bias)[1]], axis=1).astype(np.float32)
    rb = np.ascontiguousarray(np.broadcast_to(f(router_b)[0][None, :], (128, 8))).astype(np.float32)
    shared = dict(cols=cols, gb=np.ascontiguousarray(gb), rb=rb, consts=consts,
                  w_ada=f(w_ada), w_in=f(w_in), pool_w=f(pool_w), proj_a=f(proj_a), proj_b=f(proj_b),
                  w_out=f(w_out), ffn_w_gate=f(ffn_w_gate), ffn_w_up=f(ffn_w_up), ffn_w_down=f(ffn_w_down),
                  router_w=f(router_w))
    if 1 in _layers:
        shared.update(moe_w_gate=f(moe_w_gate), moe_w_up=f(moe_w_up), moe_w_down=f(moe_w_down))
    in_maps = []
    for b in range(8):
        m = dict(shared)
        m["xT"] = np.ascontiguousarray(x[b].T)
        m["cT"] = col(c[b])
        in_maps.append(m)
    with st:
        pass
    res = run_bass_kernel_spmd(nc, in_maps, core_ids=list(range(8)))
    out = np.stack([np.ascontiguousarray(res.results[b]["outT"].T) for b in range(8)], axis=0)
    return out.astype(np.float32)
```

```python
import numpy as np
from contextlib import ExitStack
import concourse.bass as bass
import concourse.mybir as mybir
from concourse.bass_utils import run_bass_kernel_spmd

F32 = mybir.dt.float32
BF16 = mybir.dt.bfloat16
AF = mybir.ActivationFunctionType
ALU = mybir.AluOpType

PE, ACT, DVE, POOL, SP = "pe", "act", "dve", "pool", "sp"
ENGS = [PE, ACT, DVE, POOL, SP]

D = 1024
T = 2048
NP = 4
TB = 512
NCH = 4
EPS = 1e-6
N_IN = 6664
DFF = 2816
DFE = 3584
NEXP = 8
LC = 140
NCOLS = 2 * LC + 8

C_ID, C_MASK, C_ONES = 0, 128, 256
C_RMUL, C_RADD, C_INVC = 384, 896, 1408
NCONST = 1408 + 64


class Prog:
    def __init__(self, nc):
        self.nc = nc
        self.ops = {e: [] for e in ENGS}
        self.last_w = {}
        self.readers = {}
        self.chan_cnt = {}
        self.nbar = 0

    def _add(self, eng, fn, reads, writes, dma_chan=None):
        pr = tuple(k for k in reads if (isinstance(k, tuple) and k and k[0] == "ps") or k in ("psb0", "psb1"))
        writes = tuple(writes) + pr
        deps = set()
        for k in reads:
            for w in self.last_w.get(k, {}).values():
                deps.add(w)
        for k in writes:
            for w in self.last_w.get(k, {}).values():
                deps.add(w)
            for r in self.readers.get(k, {}).values():
                deps.add(r)
        idx = len(self.ops[eng])
        if dma_chan is not None:
            cum = self.chan_cnt.get(dma_chan, 0) + 16
            self.chan_cnt[dma_chan] = cum
            ref = ("d", dma_chan, cum)
            who = ("q", dma_chan)
        else:
            ref = ("c", eng, idx)
            who = eng
        fdeps = []
        for d in deps:
            if d[0] == "c" and d[1] == eng:
                if eng == PE or eng == SP:
                    continue
                fdeps.append(d)
            else:
                fdeps.append(d)
        self.ops[eng].append(dict(fn=fn, deps=fdeps, ref=ref, signal=False))
        for k in reads:
            self.readers.setdefault(k, {})[who] = ref
        for k in writes:
            self.last_w.setdefault(k, {})[who] = ref
            self.readers[k] = {}
        return ref

    def op(self, eng, fn, reads=(), writes=()):
        return self._add(eng, fn, tuple(reads), tuple(writes))

    def dma(self, eng, out, in_, reads=(), writes=(), chan=None):
        def fn(e, out=out, in_=in_):
            return e.dma_start(out=out, in_=in_)
        return self._add(eng, fn, tuple(reads), tuple(writes), dma_chan=chan)

    def emit(self, stack, final_waits=()):
        nc = self.nc
        for e in ENGS:
            for op in self.ops[e]:
                for d in op["deps"]:
                    if d[0] == "c":
                        self.ops[d[1]][d[2]]["signal"] = True
        sigcount = {}
        for e in ENGS:
            c = 0
            arr = []
            for op in self.ops[e]:
                if op["signal"]:
                    c += 1
                arr.append(c)
            sigcount[e] = arr
        esem = {e: stack.enter_context(nc.semaphore("s_" + e)) for e in ENGS}
        csem = {ch: stack.enter_context(nc.semaphore("d_%d" % i))
                for i, ch in enumerate(self.chan_cnt)}
        block = stack.enter_context(nc.Block())
        engobj = {PE: "tensor", ACT: "scalar", DVE: "vector", POOL: "gpsimd", SP: "sync"}

        def make(e):
            def body(eng):
                waited = {}
                for op in self.ops[e]:
                    for d in op["deps"]:
                        if d[0] == "c":
                            sem, val, key = esem[d[1]], sigcount[d[1]][d[2]], ("c", d[1])
                        else:
                            sem, val, key = csem[d[1]], d[2], ("d", d[1])
                        if waited.get(key, 0) >= val:
                            continue
                        waited[key] = val
                        eng.wait_ge(sem, val)
                    ins = op["fn"](eng)
                    if op["ref"][0] == "d":
                        ins.then_inc(csem[op["ref"][1]], 16)
                    elif op["signal"]:
                        ins.then_inc(esem[e], 1)
                if e == SP:
                    for ch in final_waits:
                        eng.wait_ge(csem[ch], self.chan_cnt[ch])
            return body

        for e in ENGS:
            getattr(block, engobj[e])(make(e))


class _Stop(Exception):
    pass


def build(nc, layers=(0, 1), final=True, dump=None, ffn=True, stage=99):
    st = ExitStack()
    P = Prog(nc)

    def dram(name, shape, kind="ExternalInput"):
        return nc.dram_tensor(name, list(shape), F32, kind=kind).ap()

    xT_d = dram("xT", [D, T])
    cT_d = dram("cT", [128, 8])
    cols_d = dram("cols", [128, NCOLS])
    gb_d = dram("gb", [4, 4])
    rb_d = dram("rb", [128, 8])
    consts_d = dram("consts", [128, NCONST])
    w_ada = dram("w_ada", [2, D, 6 * D])
    w_in = dram("w_in", [2, D, N_IN])
    pool_w = dram("pool_w", [2, 4, 128, 128])
    proj_a = dram("proj_a", [2, D, D])
    proj_b = dram("proj_b", [2, 512, D])
    w_out = dram("w_out", [2, D, D])
    ffn_g = dram("ffn_w_gate", [1, D, DFF])
    ffn_u = dram("ffn_w_up", [1, D, DFF])
    ffn_d = dram("ffn_w_down", [1, DFF, D])
    router_w = dram("router_w", [1, D, 8])
    if 1 in layers:
        moe_g = dram("moe_w_gate", [1, NEXP, D, DFE])
        moe_u = dram("moe_w_up", [1, NEXP, D, DFE])
        moe_d = dram("moe_w_down", [1, NEXP, DFE, D])
    outT_d = dram("outT", [D, T], kind="ExternalOutput")
    scrM = nc.dram_tensor("scrM", [4, TB], F32).ap()
    scrS = nc.dram_tensor("scrS", [1, 16], F32).ap()
    dump_d = None
    if dump is not None:
        dump_d = dram("dump", dump, kind="ExternalOutput")

    def sb(name, shape, dt=F32):
        return st.enter_context(nc.sbuf_tensor(name, list(shape), dt))

    XT = sb("XT", [128, 8, T])
    CON = sb("CON", [128, NCONST])
    COLS = sb("COLS", [128, NCOLS])
    IDB = sb("IDB", [128, 128], BF16)
    ONB = sb("ONB", [128, 128], BF16)
    CACT = sb("CACT", [128, 8], BF16)
    CTF = sb("CTF", [128, 8])
    GB = sb("GB", [4, 4])
    NFB = sb("NFB", [4, 2])
    RB = sb("RB", [128, 8])
    MODC = sb("MODC", [128, 48])
    MODC2 = sb("MODC2", [128, 48])
    MODS = [MODC, MODC2]
    CUR = {"mod": MODC}
    A1 = sb("A1", [128, 8])
    A2 = sb("A2", [128, 8])
    CWH = sb("CWH", [128, 64])
    HGH = sb("HGH", [128, 8])
    G1H = sb("G1H", [128, 8])
    CST = sb("CST", [128, 4, 2, 260])
    CBF = sb("CBF", [128, 4, 2, 260], BF16)
    QTAIL = sb("QTAIL", [128, 16, 4], BF16)
    UTAIL = sb("UTAIL", [128, 4, 16])
    MCAR = sb("MCAR", [4, 1])
    WIF = sb("WIF", [128, 8, 8], BF16)
    PWB = sb("PWB", [128, 4, 128], BF16)
    WS = sb("WS", [128, 6, 4096], BF16)
    BIG = sb("BIG", [128, 72 * 256])

    ident = CON[:, C_ID:C_ID + 128]
    maskbig = CON[:, C_MASK:C_MASK + 128]

    class Arena:
        def __init__(self):
            self.off = 0

        def f32(self, n_elems, shape=None, parts=128):
            a = self.off
            self.off += n_elems
            v = BIG[0:parts, a:a + n_elems]
            return v

        def bf(self, n_elems):
            w = (n_elems + 1) // 2
            a = self.off
            self.off += w
            return BIG[:, a:a + w].bitcast(BF16)[:, 0:n_elems]

    PSF = [st.enter_context(nc.psum_tensor("psf%d" % i, [128, 512], F32)) for i in range(6)]
    PSB = [st.enter_context(nc.psum_tensor("psb%d" % i, [128, 1024], BF16)) for i in range(2)]
    psctr = [0]

    def psum():
        i = psctr[0] % 6
        psctr[0] += 1
        return PSF[i], ("ps", i)

    def mm(out_ap, pairs, reads, writes):
        pairs = list(pairs)

        def fn(e, out_ap=out_ap, pairs=pairs):
            n = len(pairs)
            ins = None
            for i, (l, r) in enumerate(pairs):
                ins = e.matmul(out_ap, lhsT=l, rhs=r, start=(i == 0), stop=(i == n - 1))
            return ins
        P.op(PE, fn, reads, writes)

    def tr(out_ap, in_ap, idn, reads, writes):
        P.op(PE, lambda e, o=out_ap, i=in_ap, d=idn: e.transpose(o, i, d), reads, writes)

    def act(out_ap, in_ap, func, reads, writes, bias=0.0, scale=1.0, accum=None):
        def fn(e, o=out_ap, i=in_ap, f=func, b=bias, s=scale, a=accum):
            if a is None:
                return e.activation(out=o, in_=i, func=f, bias=b, scale=s)
            return e.activation(out=o, in_=i, func=f, bias=b, scale=s, accum_out=a)
        P.op(ACT, fn, reads, writes)

    def tt(eng, out_ap, a, b, op, reads, writes):
        P.op(eng, lambda e, o=out_ap, a=a, b=b, op=op: e.tensor_tensor(out=o, in0=a, in1=b, op=op),
             reads, writes)

    def ts(eng, out_ap, a, s1, op0, reads, writes, s2=None, op1=None):
        def fn(e, o=out_ap, a=a, s1=s1, s2=s2, op0=op0, op1=op1):
            if op1 is None:
                return e.tensor_scalar(out=o, in0=a, scalar1=s1, scalar2=None, op0=op0)
            return e.tensor_scalar(out=o, in0=a, scalar1=s1, scalar2=s2, op0=op0, op1=op1)
        P.op(eng, fn, reads, writes)

    def stt(out_ap, a, s, b, op0, op1, reads, writes):
        P.op(DVE, lambda e, o=out_ap, a=a, s=s, b=b, op0=op0, op1=op1:
             e.scalar_tensor_tensor(out=o, in0=a, scalar=s, in1=b, op0=op0, op1=op1), reads, writes)

    def cp(eng, out_ap, in_ap, reads, writes):
        if eng == ACT:
            P.op(ACT, lambda e, o=out_ap, i=in_ap: e.copy(out=o, in_=i), reads, writes)
        else:
            P.op(eng, lambda e, o=out_ap, i=in_ap: e.tensor_copy(out=o, in_=i), reads, writes)

    def scan(out_ap, d0, d1, init, op0, op1, reads, writes):
        P.op(DVE, lambda e, o=out_ap, d0=d0, d1=d1, i=init, op0=op0, op1=op1:
             e.tensor_tensor_scan(out=o, data0=d0, data1=d1, initial=i, op0=op0, op1=op1),
             reads, writes)

    def memset(eng, ap, val, writes):
        P.op(eng, lambda e, a=ap, v=val: e.memset(a, v), (), writes)

    ALLK = []

    def barrier():
        n = P.nbar
        P.nbar += 1
        for e in (PE, ACT, DVE, POOL):
            pass
        keys = list(P.last_w.keys())
        rkeys = list(P.readers.keys())
        allk = list(set(keys) | set(rkeys))
        P.op(DVE, lambda e: e.memset(BARS[:, 0:1], 0.0), (), allk)
        for e in (ACT, POOL, PE, SP):
            P.op(e, lambda x: x.nop(nofuse=True), allk, ())

    BARS = sb("BARS", [128, 4])

    wslot_ctr = [0]

    def wslot(n=1):
        nslots = 6
        i = wslot_ctr[0]
        if (i % nslots) + n > nslots:
            i += nslots - (i % nslots)
        wslot_ctr[0] = i + n
        return [(i + j) % nslots for j in range(n)]

    def wview(slots, shape_str, **kw):
        s0 = slots[0]
        n = len(slots)
        v = WS[:, s0:s0 + n, :].rearrange("p s n -> p (s n)")
        return v.rearrange(shape_str, **kw)

    def wkeys(slots):
        return [("ws", s) for s in slots]

    def wdma(dst_ap, src_ap, slots):
        P.dma(POOL, dst_ap, src_ap, writes=wkeys(slots), chan=("ws", slots[0]))

    P.dma(SP, CON[:], consts_d, writes=["con"], chan="con")
    P.dma(SP, COLS[:], cols_d, writes=["cols"], chan="cols")
    P.dma(SP, CTF[:], cT_d, writes=["ctf"], chan="misc")
    P.dma(SP, GB[:], gb_d, writes=["gb"], chan="misc2")
    P.dma(SP, RB[:], rb_d, writes=["rb"], chan="misc3")
    for kc in range(8):
        P.dma(SP, XT[:, kc, :], xT_d[kc * 128:(kc + 1) * 128, :], writes=[("x", kc, p) for p in range(NP)],
              chan=("xload", kc))
    cp(DVE, IDB[:], ident, ["con"], ["idb"])
    memset(DVE, ONB[:], 1.0, ["onb"])
    act(CACT[:], CTF[:], AF.Silu, ["ctf"], ["cact"])
    ts(DVE, NFB[:, 0:1], GB[:, 1:2], -1.0, ALU.mult, ["gb"], ["nfb"])
    ts(DVE, NFB[:, 1:2], GB[:, 3:4], -1.0, ALU.mult, ["gb"], ["nfb"])

    ddump = {"n": 0}
    NTMP = {}

    WQ = {"reqs": [], "idx": {}, "issued": 0, "views": {}, "live": {}, "cur": 0}

    def wq_add(key, fn, nsl):
        WQ["idx"][key] = len(WQ["reqs"])
        WQ["reqs"].append((key, fn, nsl))

    def peek_wslot(n):
        nslots = 6
        i = wslot_ctr[0]
        if (i % nslots) + n > nslots:
            i += nslots - (i % nslots)
        return [(i + j) % nslots for j in range(n)]

    def wq_issue_upto(i, strict):
        i = min(i, len(WQ["reqs"]) - 1)
        while WQ["issued"] <= i:
            key, fn, nsl = WQ["reqs"][WQ["issued"]]
            slots = peek_wslot(nsl)
            busy = set()
            for v in WQ["live"].values():
                busy.update(v)
            if any(s_ in busy for s_ in slots):
                assert not strict, ("weight slot conflict", key, slots, WQ["live"])
                return
            WQ["views"][key] = fn()
            WQ["live"][key] = slots
            WQ["issued"] += 1

    def need(key):
        i = WQ["idx"][key]
        wq_issue_upto(i, True)
        WQ["cur"] = max(WQ["cur"], i)
        return WQ["views"][key]

    def prefetch(n):
        wq_issue_upto(WQ["cur"] + n, False)

    def release(*keys):
        for k in keys:
            WQ["live"].pop(k, None)

    def rmsnorm_mod(l, src_keys_fn, p, Acol, shcol_base, dst_fn, dst_key, ar, tagp):
        sq = [ar.bf(TB) for _ in range(2)]
        rr = ar.f32(TB)
        t0_ = ar.off
        tmp = [ar.f32(TB) for _ in range(2)]
        NTMP["rr"] = rr
        NTMP["sq0"] = sq[0]
        NTMP["tmp01"] = BIG[:, t0_:t0_ + 2 * TB]
        pt, pk = psum()
        for kc in range(8):
            act(sq[kc % 2], XT[:, kc, p * TB:(p + 1) * TB], AF.Square, [("x", kc, p)], [(tagp, "sq", kc % 2)])
            P.op(PE, lambda e, kc=kc, pt=pt, s=sq[kc % 2]: e.matmul(pt[:], lhsT=ONB[:], rhs=s,
                                                                     start=(kc == 0), stop=(kc == 7)),
                 [(tagp, "sq", kc % 2), "onb"], [pk])
        ts(DVE, rr, pt[:], 1024.0 * EPS, ALU.add, [pk], [(tagp, "rr")])
        act(rr, rr, AF.Ln, [(tagp, "rr")], [(tagp, "rr")])
        act(rr, rr, AF.Exp, [(tagp, "rr")], [(tagp, "rr")], scale=-0.5)
        for kc in range(8):
            stt(tmp[kc % 2], XT[:, kc, p * TB:(p + 1) * TB], Acol[:, kc:kc + 1], rr, ALU.mult, ALU.mult,
                [("x", kc, p), (tagp, "rr"), "modc"], [(tagp, "tmp", kc % 2)])
            act(dst_fn(kc), tmp[kc % 2], AF.Identity, [(tagp, "tmp", kc % 2), "modc"], [dst_key],
                bias=CUR["mod"][:, shcol_base + kc:shcol_base + kc + 1])

    def ada_blocks_now(l, blks):
        MOD = MODS[l]
        c0 = l * LC
        pt, pk = psum()
        for blk in blks:
            sl = wslot(1)
            wv = wview(sl, "p (k n) -> p k n", k=8)
            wdma(wv, w_ada[l][:, blk * 512:(blk + 1) * 512].rearrange("(kc p) n -> p kc n", p=128), sl)
            for jj in range(4):
                j = blk * 4 + jj
                mm(pt[:, j:j + 1], [(wv[:, kc, jj * 128:(jj + 1) * 128], CACT[:, kc:kc + 1]) for kc in range(8)],
                   wkeys(sl) + ["cact"], [pk])
        lo, hi = 4 * min(blks), 4 * max(blks) + 4
        tt(DVE, MOD[:, lo:hi], pt[:, lo:hi], COLS[:, c0 + 16 + lo:c0 + 16 + hi], ALU.add, [pk, "cols"], ["modc"])

    def derive_early(l):
        MOD = MODS[l]
        c0 = l * LC
        ts(DVE, A1[:], MOD[:, 8:16], 1.0, ALU.add, ["modc", ("mod", l)], ["a1"], s2=32.0, op1=ALU.mult)
        tt(DVE, A1[:], A1[:], COLS[:, c0:c0 + 8], ALU.mult, ["a1", "cols"], ["modc", ("mod", l)])
        ts(DVE, CWH[:], COLS[:, c0 + 64:c0 + 128], 0.5, ALU.mult, ["cols"], ["cwh"])
        ts(DVE, HGH[:], COLS[:, c0 + 132:c0 + 140], 0.5, ALU.mult, ["cols"], ["hgh"])

    def derive_late(l):
        MOD = MODS[l]
        c0 = l * LC
        ts(DVE, G1H[:], MOD[:, 16:24], 0.5, ALU.mult, ["modc", ("mod", l)], ["g1h"])
        ts(DVE, A2[:], MOD[:, 32:40], 1.0, ALU.add, ["modc", ("mod", l)], ["a2"], s2=32.0, op1=ALU.mult)
        tt(DVE, A2[:], A2[:], COLS[:, c0 + 8:c0 + 16], ALU.mult, ["a2", "cols"], ["modc", ("mod", l)])

    def ada_block_deferred(lsrc, blk, psum_fn):
        MOD = MODS[lsrc]
        c0s = lsrc * LC
        sl, wv = need(("ada", lsrc, blk))
        pt, pk = psum_fn()
        for jj in range(4):
            mm(pt[:, jj:jj + 1], [(wv[:, kc, jj * 128:(jj + 1) * 128], CACT[:, kc:kc + 1]) for kc in range(8)],
               wkeys(sl) + ["cact"], [pk])
        tt(DVE, MOD[:, 4 * blk:4 * blk + 4], pt[:, 0:4], COLS[:, c0s + 16 + 4 * blk:c0s + 16 + 4 * blk + 4], ALU.add,
           [pk, "cols"], [("mod", lsrc)])
        release(("ada", lsrc, blk))

    def mixer_layer(l):
        c0 = l * LC
        P.dma(POOL, WIF[:], w_in[l][:, 4096:4104].rearrange("(kc p) n -> p kc n", p=128), writes=["wif"], chan="wif")
        P.dma(POOL, PWB[:], pool_w[l].rearrange("g c d -> c g d"), writes=["pwb"], chan="pwb")
        memset(DVE, CST[:], 0.0, ["cst"])
        memset(DVE, CBF[:], 0.0, [("cbf", h_) for h_ in range(4)])
        memset(DVE, QTAIL[:], 0.0, ["qtail"])
        memset(DVE, UTAIL[:], 0.0, ["utail"])
        memset(DVE, MCAR[:], 0.0, ["mcar"])
        cbf_par = [0]

        def req_head(h):
            def fn():
                sl = wslot(2)
                whd = wview(sl, "p (k j c) -> p k j c", k=8, j=4)
                for j4 in range(4):
                    wdma(whd[:, :, j4, :], w_in[l][:, j4 * 1024 + h * 256:j4 * 1024 + (h + 1) * 256].rearrange("(kc p) c -> p kc c", p=128), sl)
                return sl, whd
            return fn

        def req_cols(src, c_lo, ncols, kcn=8):
            def fn():
                sl = wslot(1)
                wv = wview(sl, "p (k n) -> p k n", k=8)
                wdma(wv[:, 0:kcn, 0:ncols], src[:, c_lo:c_lo + ncols].rearrange("(kc p) n -> p kc n", p=128), sl)
                return sl, wv
            return fn

        def req_wout():
            sl = wslot(2)
            wv = wview(sl, "p (k n) -> p k n", k=8)
            wdma(wv, w_out[l].rearrange("(kc p) n -> p kc n", p=128), sl)
            return sl, wv

        ADA = {}
        if l == layers[0]:
            for i_, blk in enumerate(range(4, 12)):
                ADA.setdefault((0, i_ // 2), []).append((l, blk))
            if len(layers) > 1:
                for blk in range(12):
                    ADA.setdefault((1 + blk // 4, blk % 4), []).append((layers[1], blk))
        for p in range(NP):
            for h in range(4):
                wq_add(("head", p, h), req_head(h), 2)
                for (ls_, blk_) in ADA.get((p, h), []):
                    wq_add(("ada", ls_, blk_), req_cols(w_ada[ls_], blk_ * 512, 512), 1)
            wq_add(("u", p), req_cols(w_in[l], 4104, 512), 1)
            for grp in range(2):
                wq_add(("ga", p, grp), req_cols(w_in[l], 4616 + grp * 512, 512), 1)
                wq_add(("pa", p, grp), req_cols(proj_a[l], grp * 512, 512), 1)
            for grp in range(2):
                wq_add(("gb", p, grp), req_cols(w_in[l], 5640 + grp * 512, 512), 1)
                wq_add(("pb", p, grp), req_cols(proj_b[l], grp * 512, 512, kcn=4), 1)
            wq_add(("wo", p), req_wout, 2)

        for p in range(NP):
            tsl = slice(p * TB, (p + 1) * TB)
            if p == 0:
                barrier()
            ar = Arena()
            HP = ar.bf(8 * TB).rearrange("p (k t) -> p k t", k=8)
            HAT = ar.bf(8 * TB).rearrange("p (k t) -> p k t", k=8)
            HBT = ar.bf(4 * TB).rearrange("p (k t) -> p k t", k=4)
            rmsnorm_mod(l, None, p, A1, 0, lambda kc: HP[:, kc, :], "hp", ar, "n1")
            if p > 0:
                barrier()
            chk(3)
            SM = ar.f32(64, parts=4)
            nbt, gmx, mloc, btot, mnext, mprev, d1, sold = [SM[:, i * 4:(i + 1) * 4] for i in range(8)]
            GCOL = ar.f32(64)
            SOLDB = ar.f32(16)
            MB = ar.f32(TB)
            HEADMARK = ar.off
            R32 = [ar.f32(TB, parts=32) for _ in range(5)]
            R = [r_[0:4, :] for r_ in R32]
            pi, pik = psum()
            pf, pfk = psum()
            mm(pi[0:4, :], [(WIF[:, kc, 0:4], HP[:, kc, :]) for kc in range(8)], ["wif", "hp"], [pik])
            mm(pf[0:4, :], [(WIF[:, kc, 4:8], HP[:, kc, :]) for kc in range(8)], ["wif", "hp"], [pfk])
            rA, rB, rC, rD, rE = R
            ts(DVE, rA, pi[0:4, :], GB[:, 2 * l:2 * l + 1], ALU.add, [pik, "gb"], ["rA"])
            act(rB, pf[0:4, :], AF.Exp, [pfk, "nfb"], ["rB"], bias=NFB[:, l:l + 1], scale=-1.0)
            act(rB, rB, AF.Ln, ["rB"], ["rB"], bias=1.0)
            chk(3.1)
            scan(rC, CON[0:4, C_RMUL:C_RMUL + TB], rB, 0.0, ALU.mult, ALU.add, ["rB", "con"], ["rC"])
            tt(DVE, rA, rA, rC, ALU.add, ["rA", "rC"], ["rA"])
            scan(rD, CON[0:4, C_RADD:C_RADD + TB], rA, 0.0, ALU.add, ALU.max, ["rA", "con"], ["rD"])
            chk(3.2)
            nbt1, gmx1, mloc1, btot1, mnext1, mprev1, d11, sold1 = [x_[:, 0:1] for x_ in (nbt, gmx, mloc, btot, mnext, mprev, d1, sold)]
            cp(DVE, nbt1, rC[:, TB - 1:TB], ["rC"], ["sm"])
            cp(DVE, gmx1, rD[:, TB - 1:TB], ["rD"], ["sm"])
            tt(DVE, mloc1, gmx1, nbt1, ALU.subtract, ["sm"], ["sm"])
            ts(DVE, btot1, nbt1, -1.0, ALU.mult, ["sm"], ["sm"])
            tt(DVE, mnext1, btot1, MCAR[:, 0:1], ALU.add, ["sm", "mcar"], ["sm"])
            tt(DVE, mnext1, mnext1, mloc1, ALU.max, ["sm"], ["sm"])
            cp(DVE, mprev1, MCAR[:, 0:1], ["mcar", "sm"], ["sm"])
            cp(DVE, MCAR[:, 0:1], mnext1, ["sm"], ["mcar"])
            tt(DVE, d11, btot1, mnext1, ALU.subtract, ["sm"], ["sm"])
            tt(DVE, sold1, d11, mprev1, ALU.add, ["sm"], ["sm"])
            act(sold1, sold1, AF.Exp, ["sm"], ["sm"])
            chk(3.3)
            ts(DVE, rD, rD, mprev1, ALU.max, ["rD", "sm"], ["rD"])
            ts(DVE, rB, rD, mprev1, ALU.subtract, ["rD", "sm", "rB"], ["rB"])
            act(rB, rB, AF.Exp, ["rB"], ["rB"], bias=float(-np.log(16.0)), scale=-1.0)
            tt(DVE, rC, rC, rD, ALU.subtract, ["rC", "rD"], ["rC"])
            act(rC, rC, AF.Exp, ["rC"], ["rC"])
            ts(DVE, rE, rA, d11, ALU.add, ["rA", "sm"], ["rE"])
            act(rE, rE, AF.Exp, ["rE"], ["rE"])
            chk(3.4)
            pg, pgk = psum()
            for qi, (rr_, rk) in enumerate([(R32[0], "rA"), (R32[4], "rE"), (R32[2], "rC"), (R32[1], "rB")]):
                for c in range(NCH):
                    a_ = qi * 4 + c
                    tr(pg[:, a_ * 32:(a_ + 1) * 32], rr_[:, c * 128:(c + 1) * 128], ident[0:32, 0:32], [rk, "con"], [pgk])
            cp(DVE, GCOL.rearrange("p (a h) -> p a h", h=4), pg[:].rearrange("p (a b) -> p a b", b=32)[:, :, 0:4],
               [pgk], ["gcol"])
            chk(3.5)
            P.dma(SP, scrS[:, 0:4].rearrange("o (h c) -> (o h) c", h=4), sold1, reads=["sm"], writes=["scrS"], chan="scrS")
            P.dma(SP, SOLDB[:, 0:4], scrS[:, 0:4].partition_broadcast(128), reads=["scrS"], writes=["soldb"], chan="soldb")
            P.dma(SP, scrM, rD, reads=["rD"], writes=["scrM"], chan="scrM")
            barrier()
            ar.off = HEADMARK

            chk(4)
            RAWB = ar.bf(TB + 4)
            T5 = ar.f32(TB)
            DG = [ar.bf(128) for _ in range(4)]
            HB = []
            for par_ in range(2):
                HB.append(dict(
                    QT=ar.bf(2 * TB).rearrange("p (k t) -> p k t", k=2),
                    KT=ar.bf(2 * TB).rearrange("p (k t) -> p k t", k=2),
                    KTM=ar.bf(NCH * 256).rearrange("p (c f) -> p c f", c=NCH),
                    VTM=ar.bf(NCH * 256).rearrange("p (c f) -> p c f", c=NCH),
                    OG=ar.bf(NCH * 256).rearrange("p (c f) -> p c f", c=NCH),
                    k=par_))
            DT4 = ar.f32(1280)
            PTX = ar.bf(1280 - 512)
            VW4 = ar.bf(NCH * 256).rearrange("p (c f) -> p c f", c=NCH)
            WHB = ar.bf(4)
            ND = ar.f32(NCH * 256).rearrange("p (c f) -> p c f", c=NCH)
            JUNK = ar.bf(256)
            HATM = ar.bf(NCH * 256).rearrange("p (c f) -> p c f", c=NCH)
            SMALL = ar.f32(64)
            SIGT = [ar.f32(256) for _ in range(2)]
            TMPD4 = NTMP["rr"]
            PT4 = NTMP["sq0"]
            ND1 = NTMP["tmp01"].rearrange("p (c f) -> p c f", c=NCH)
            K_RR, K_SQ0, K_T0, K_T1 = ("n1", "rr"), ("n1", "sq", 0), ("n1", "tmp", 0), ("n1", "tmp", 1)
            pj_ctr = [0]

            def psum_pj():
                i = 4 + (pj_ctr[0] % 2)
                pj_ctr[0] += 1
                return PSF[i], ("ps", i)

            def c3(a, n):
                return a.rearrange("p (c t) -> p c t", c=NCH)

            def proj_gen(h, B):
                QT, KT, KTM, VTM, OG = B["QT"], B["KT"], B["KTM"], B["VTM"], B["OG"]
                bq = B["k"]
                kq, kk, kkt, kv, ko = ("qt", bq), ("kt", bq), ("ktm", bq), ("vtm", bq), ("og", bq)
                sl, whd = need(("head", p, h))
                prefetch(2)
                for qk, dstT, dkey in ((0, QT, kq), (1, KT, kk)):
                    for fc in range(2):
                        j = qk * 8 + h * 2 + fc
                        pt, pk = psum_pj()
                        mm(pt[:], [(whd[:, kc, qk, fc * 128:(fc + 1) * 128], HP[:, kc, :]) for kc in range(8)],
                           wkeys(sl) + ["hp"], [pk])
                        cp(DVE, RAWB[:, 0:3], QTAIL[:, j, 0:3], ["qtail"], ["rawb"])
                        cp(ACT, RAWB[:, 3:3 + TB], pt[:], [pk], ["rawb"])
                        cw = CWH[:, j * 4:j * 4 + 4]
                        for tap in range(4):
                            ts(DVE, DG[tap], ident, cw[:, tap:tap + 1], ALU.mult, ["con", "cwh"], [("dg", tap)])
                        yield
                        py, pyk = psum_pj()
                        mm(py[:], [(DG[tap], RAWB[:, tap:tap + TB]) for tap in range(4)],
                           [("dg", t_) for t_ in range(4)] + ["rawb"], [pyk])
                        cp(DVE, QTAIL[:, j, 0:3], RAWB[:, TB:TB + 3], ["rawb"], ["qtail"])
                        yield
                        act(T5, py[:], AF.Tanh, [pyk], ["t5"])
                        yield
                        stt(dstT[:, fc, :], T5, 1.0, py[:], ALU.add, ALU.mult, ["t5", pyk], [dkey])
                        yield
                for fc in range(2):
                    for c in range(NCH):
                        tr(PSB[0][:, c * 256 + fc * 128:c * 256 + (fc + 1) * 128], KT[:, fc, c * 128:(c + 1) * 128],
                           IDB[:], [kk, "idb"], ["psb0"])
                cp(ACT, KTM.rearrange("p c f -> p (c f)"), PSB[0][:, 0:1024], ["psb0"], [kkt])
                yield
                for c in range(NCH):
                    pt, pk = psum_pj()
                    mm(pt[:], [(HP[:, kc, c * 128:(c + 1) * 128], whd[:, kc, 2:4, :].rearrange("p j c -> p (j c)"))
                               for kc in range(8)], wkeys(sl) + ["hp"], [pk])
                    cp(DVE, VTM[:, c, :], pt[:, 0:256], [pk], [kv])
                    act(SIGT[c % 2], pt[:, 256:512], AF.Tanh, [pk], [("sigt", c % 2)], scale=0.5)
                    ts(DVE, OG[:, c, :], SIGT[c % 2], 1.0, ALU.add, [("sigt", c % 2)], [ko])
                    yield
                release(("head", p, h))
                for (ls_, blk_) in ADA.get((p, h), []):
                    ada_block_deferred(ls_, blk_, psum_pj)
                    yield
                prefetch(3)

            def chunk_gen(h, B):
                QT, KT, KTM, VTM, OG = B["QT"], B["KT"], B["KTM"], B["VTM"], B["OG"]
                bq = B["k"]
                kq, kk, kkt, kv, ko = ("qt", bq), ("kt", bq), ("ktm", bq), ("vtm", bq), ("og", bq)
                if h == 0:
                    P.dma(SP, MB, scrM[0:1, :].partition_broadcast(128), reads=["scrM"], writes=["mb"], chan="mb")
                wh4 = GCOL[:, 16 + h:32:4]
                df4 = GCOL[:, 32 + h:48:4]
                wi4 = GCOL[:, 48 + h:64:4]
                bk = lambda i: ("ps", i)
                WJ = [512, 384, 256, 128]
                SREG = [(0, 0), (1, 0), (3, 128), (1, 384)]
                DOFF = [0, 512, 896, 1152]
                for j in range(NCH):
                    bnk, co = SREG[j]
                    mm(PSF[bnk][:, co:co + WJ[j]],
                       [(KT[:, kc, j * 128:(j + 1) * 128], QT[:, kc, j * 128:TB]) for kc in range(2)], [kk, kq], [bk(bnk)])
                tt(DVE, VW4, VTM, wh4.unsqueeze(2).to_broadcast([128, NCH, 256]), ALU.mult, [kv, "gcol"], ["vw"])
                cp(DVE, WHB, wh4, ["gcol"], ["whb"])
                yield
                for kc in range(2):
                    mm(PSF[2][:, kc * 256:(kc + 1) * 256],
                       [(KTM[:, c, kc * 128:(kc + 1) * 128], VW4[:, c, :]) for c in range(NCH)], [kkt, "vw"], [bk(2)])
                for kc in range(2):
                    mm(PSF[3][:, 8 + kc:9 + kc],
                       [(KTM[:, c, kc * 128:(kc + 1) * 128], WHB[:, c:c + 1]) for c in range(NCH)], [kkt, "whb"], [bk(3)])
                tt(DVE, c3(TMPD4, 4), c3(MB, 4), maskbig.unsqueeze(1).to_broadcast([128, NCH, 128]), ALU.add,
                   ["mb", "con", K_RR], [K_RR, "tmpd"])
                yield
                for j in range(NCH):
                    gcol = GCOL[:, j * 4 + h:j * 4 + h + 1]
                    act(DT4[:, DOFF[j]:DOFF[j] + 128], TMPD4[:, j * 128:(j + 1) * 128], AF.Exp, ["tmpd", "gcol"], ["dt"],
                        bias=gcol, scale=-1.0)
                    if j < 3:
                        act(DT4[:, DOFF[j] + 128:DOFF[j] + WJ[j]], MB[:, (j + 1) * 128:TB], AF.Exp, ["mb", "gcol"], ["dt"],
                            bias=gcol, scale=-1.0)
                    yield
                if h < 3:
                    P.dma(SP, MB, scrM[h + 1:h + 2, :].partition_broadcast(128), reads=["scrM"], writes=["mb"], chan="mb")
                PT = [PT4[:, 0:512], PTX[:, 0:384], PTX[:, 384:640], PTX[:, 640:768]]
                for j in range(NCH):
                    bnk, co = SREG[j]
                    stt(PT[j], PSF[bnk][:, co:co + WJ[j]], 1.0 / 16.0, DT4[:, DOFF[j]:DOFF[j] + WJ[j]], ALU.mult, ALU.mult,
                        [bk(bnk), "dt", K_SQ0], [K_SQ0, "pt"])
                yield
                sb_ = SOLDB[:, h:h + 1]
                stt(CST[:, h, :, 0:256], CST[:, h, :, 0:256], sb_, PSF[2][:].rearrange("p (k f) -> p k f", k=2),
                    ALU.mult, ALU.add, ["cst", "soldb", bk(2)], ["cst"])
                stt(CST[:, h, :, 256:257], CST[:, h, :, 256:257], sb_,
                    PSF[3][:, 8:10].rearrange("p (k f) -> p k f", k=2),
                    ALU.mult, ALU.add, ["cst", "soldb", bk(3)], ["cst"])
                yield
                for c in range(NCH):
                    bnk = 0 if c < 2 else 1
                    mm(PSF[bnk][:, (c % 2) * 256:(c % 2 + 1) * 256],
                       [(PT[j][:, (c - j) * 128:(c - j + 1) * 128], VTM[:, j, :]) for j in range(c + 1)], ["pt", kv], [bk(bnk)])
                for c in range(NCH):
                    mm(PSF[3][:, c:c + 1],
                       [(PT[j][:, (c - j) * 128:(c - j + 1) * 128], ONB[:, 0:1]) for j in range(c + 1)], ["pt", "onb"], [bk(3)])
                cp(ACT, ND1[:, 0:2, :].rearrange("p c f -> p (c f)"), PSF[0][:], [bk(0), K_T0, K_T1], [K_T0, K_T1, "nd1"])
                cp(ACT, ND1[:, 2:4, :].rearrange("p c f -> p (c f)"), PSF[1][:], [bk(1), K_T0, K_T1], [K_T0, K_T1, "nd1"])
                yield
                for c in range(NCH):
                    csl = slice(c * 128, (c + 1) * 128)
                    bnk = 0 if c < 2 else 1
                    mm(PSF[bnk][:, (c % 2) * 256:(c % 2 + 1) * 256],
                       [(QT[:, kc, csl], CBF[:, h, kc, 0:256]) for kc in range(2)], [kq, ("cbf", h)], [bk(bnk)])
                for c in range(NCH):
                    csl = slice(c * 128, (c + 1) * 128)
                    mm(PSF[3][:, 4 + c:5 + c], [(QT[:, kc, csl], CBF[:, h, kc, 256:257]) for kc in range(2)],
                       [kq, ("cbf", h)], [bk(3)])
                yield
                for c in range(NCH):
                    bnk = 0 if c < 2 else 1
                    stt(ND[:, c, :], PSF[bnk][:, (c % 2) * 256:(c % 2 + 1) * 256], GCOL[:, 48 + c * 4 + h:48 + c * 4 + h + 1],
                        ND1[:, c, :], ALU.mult, ALU.add, [bk(bnk), "nd1", "gcol", K_T0, K_T1], ["nd"])
                    if c % 2 == 1:
                        yield
                cp(ACT, CBF[:, h, :, 0:257], CST[:, h, :, 0:257], ["cst"], [("cbf", h)])
                DEN8, DEN, dd, r1, t1, u1, scl, ssq = [SMALL[:, i * 8:i * 8 + 4] for i in range(8)]
                cp(DVE, SMALL[:, 0:8], PSF[3][:, 0:8], [bk(3)], ["small"])
                tt(DVE, DEN, SMALL[:, 4:8], wi4, ALU.mult, ["small", "gcol"], ["small"])
                tt(DVE, DEN, DEN, SMALL[:, 0:4], ALU.add, ["small"], ["small"])
                for c in range(NCH):
                    act(JUNK, ND[:, c, :], AF.Square, ["nd"], ["junk", "small_ssq"], accum=ssq[:, c:c + 1])
                yield
                ts(DVE, dd, DEN, -1.0, ALU.mult, ["small"], ["small"])
                tt(DVE, dd, dd, DEN, ALU.max, ["small"], ["small"])
                yield
                tt(DVE, dd, dd, df4, ALU.max, ["gcol", "small"], ["small"])
                P.op(DVE, lambda e, o=r1, i=dd: e.reciprocal(out=o, in_=i), ["small"], ["small"])
                yield
                tt(DVE, t1, ssq, r1, ALU.mult, ["small", "small_ssq"], ["small"])
                tt(DVE, t1, t1, r1, ALU.mult, ["small"], ["small"])
                yield
                ts(DVE, u1, t1, 1.0 / 256.0, ALU.mult, ["small"], ["small"], s2=EPS, op1=ALU.add)
                act(u1, u1, AF.Ln, ["small"], ["small_u"])
                yield
                act(u1, u1, AF.Exp, ["small_u"], ["small_u"], scale=-0.5)
                tt(DVE, scl, u1, r1, ALU.mult, ["small", "small_u"], ["small"])
                yield
                for c in range(NCH):
                    stt(HATM[:, c, :], ND[:, c, :], scl[:, c:c + 1], OG[:, c, :], ALU.mult, ALU.mult,
                        ["nd", "small", ko], ["hatm"])
                yield
                for c in range(NCH):
                    for fc in range(2):
                        tr(PSB[1][:, fc * 512 + c * 128:fc * 512 + (c + 1) * 128], HATM[:, c, fc * 128:(fc + 1) * 128],
                           IDB[:], ["hatm", "idb"], ["psb1"])
                for fc in range(2):
                    kcg = 2 * h + fc
                    act(HAT[:, kcg, :], PSB[1][:, fc * 512:(fc + 1) * 512], AF.Identity, ["psb1", "hgh"], ["hat"],
                        scale=HGH[:, kcg:kcg + 1])

            def run_interleaved(gens):
                gens = list(gens)
                while gens:
                    for g_ in list(gens):
                        try:
                            next(g_)
                        except StopIteration:
                            gens.remove(g_)

            run_interleaved([proj_gen(0, HB[0])])
            for h in range(4):
                gl = [chunk_gen(h, HB[h % 2])]
                if h < 3:
                    gl.append(proj_gen(h + 1, HB[(h + 1) % 2]))
                run_interleaved(gl)
            if l == layers[0] and p == 0:
                derive_late(l)

            chk(5)
            barrier()
            ar.off = HEADMARK
            UB = ar.f32(TB + 16)
            LV = [ar.f32(TB + 16) for _ in range(2)]
            PLD = ar.bf(TB)
            T16 = ar.f32(16)
            MG = ar.bf(8 * TB).rearrange("p (k t) -> p k t", k=8)
            SGA = ar.f32(TB)
            SGB = ar.f32(TB)
            M2 = ar.f32(TB)

            def pool_gen():
                sl, wu = need(("u", p))
                for g in range(4):
                    pt, pk = psum()
                    mm(pt[:], [(wu[:, kc, g * 128:(g + 1) * 128], HP[:, kc, :]) for kc in range(8)], wkeys(sl) + ["hp"], [pk])
                    cp(DVE, UB[:, 0:16], UTAIL[:, g, :], ["utail"], ["ub"])
                    cp(ACT, UB[:, 16:16 + TB], pt[:], [pk], ["ub"])
                    cp(DVE, UTAIL[:, g, :], UB[:, TB:TB + 16], ["ub"], ["utail"])
                    yield
                    src, skey = UB, "ub"
                    d = 1
                    for lev in range(g + 1):
                        dst = LV[lev % 2]
                        dk = ("lv", lev % 2)
                        lo = 2 * d - 1
                        tt(DVE, dst[:, lo:TB + 16], src[:, lo:TB + 16], src[:, lo - d:TB + 16 - d], ALU.add,
                           [skey], [dk])
                        src, skey = dst, dk
                        d *= 2
                        yield
                    win = float(d)
                    stt(PLD, src[:, 16:16 + TB], 1.0 / win, UB[:, 16:16 + TB], ALU.mult, ALU.subtract,
                        [skey, "ub"], ["pld"])
                    if p == 0:
                        tt(DVE, T16, src[:, 16:32], CON[:, C_INVC + g * 16:C_INVC + (g + 1) * 16], ALU.mult,
                           [skey, "con"], ["t16"])
                        tt(DVE, PLD[:, 0:16], T16, UB[:, 16:32], ALU.subtract, ["t16", "ub", "pld"], ["pld"])
                    yield
                    pt2, pk2 = psum()
                    mm(pt2[:], [(PWB[:, g, :], PLD)], ["pwb", "pld"], [pk2])
                    ts(DVE, HBT[:, g, :], pt2[:], COLS[:, c0 + 128 + g:c0 + 128 + g + 1], ALU.mult, [pk2, "cols"], ["hbt"])
                    yield
                release(("u", p))

            def merge_a_gen():
                for grp in range(2):
                    s_ga, wga = need(("ga", p, grp))
                    s_pa, wpa = need(("pa", p, grp))
                    for dd_ in range(4):
                        dc = grp * 4 + dd_
                        cs = slice(dd_ * 128, (dd_ + 1) * 128)
                        pga, pgak = psum()
                        ppa, ppak = psum()
                        mm(pga[:], [(wga[:, kc, cs], HP[:, kc, :]) for kc in range(8)], wkeys(s_ga) + ["hp"], [pgak])
                        mm(ppa[:], [(wpa[:, kc, cs], HAT[:, kc, :]) for kc in range(8)], wkeys(s_pa) + ["hat"], [ppak])
                        act(SGA, pga[:], AF.Tanh, [pgak], ["sga"], scale=0.5)
                        yield
                        stt(MG[:, dc, :], SGA, 1.0, ppa[:], ALU.add, ALU.mult, [ppak, "sga"], [("mg", dc)])
                        yield
                    release(("ga", p, grp), ("pa", p, grp))
                    prefetch(2)

            run_interleaved([pool_gen(), merge_a_gen()])
            chk(6)
            for grp in range(2):
                s_gb, wgb = need(("gb", p, grp))
                s_pb, wpb = need(("pb", p, grp))
                prefetch(2)
                for dd_ in range(4):
                    dc = grp * 4 + dd_
                    cs = slice(dd_ * 128, (dd_ + 1) * 128)
                    pgb, pgbk = psum()
                    ppb, ppbk = psum()
                    mm(pgb[:], [(wgb[:, kc, cs], HP[:, kc, :]) for kc in range(8)], wkeys(s_gb) + ["hp"], [pgbk])
                    mm(ppb[:], [(wpb[:, kc, cs], HBT[:, kc, :]) for kc in range(4)], wkeys(s_pb) + ["hbt"], [ppbk])
                    act(SGB, pgb[:], AF.Tanh, [pgbk], ["sgb"], scale=0.5)
                    stt(M2, SGB, 1.0, ppb[:], ALU.add, ALU.mult, [ppbk, "sgb"], ["m2"])
                    tt(DVE, MG[:, dc, :], MG[:, dc, :], M2, ALU.add, ["m2", ("mg", dc)], [("mg", dc)])
                release(("gb", p, grp), ("pb", p, grp))
                prefetch(2)
            s_wo, wwo = need(("wo", p))
            prefetch(2)
            for dc in range(8):
                pt, pk = psum()
                mm(pt[:], [(wwo[:, kc, dc * 128:(dc + 1) * 128], MG[:, kc, :]) for kc in range(8)],
                   wkeys(s_wo) + [("mg", k_) for k_ in range(8)], [pk])
                stt(XT[:, dc, tsl], pt[:], G1H[:, dc:dc + 1], XT[:, dc, tsl], ALU.mult, ALU.add,
                    [pk, "g1h", ("x", dc, p)], [("x", dc, p)])
            release(("wo", p))

    FIN = {"done": False, "oc": 0}

    def final_norm_pass(p, fb):
        fcol = COLS[:, 2 * LC:2 * LC + 8]
        sq, rr, OUTB = fb["sq"], fb["rr"], fb["OUTB"]
        tsl = slice(p * TB, (p + 1) * TB)
        pt, pk = psum()
        for kc in range(8):
            act(sq[kc % 2], XT[:, kc, tsl], AF.Square, [("x", kc, p)], [("fsq", kc % 2)])
            P.op(PE, lambda e, kc=kc, pt=pt, s_=sq[kc % 2]: e.matmul(pt[:], lhsT=ONB[:], rhs=s_,
                                                                      start=(kc == 0), stop=(kc == 7)),
                 [("fsq", kc % 2), "onb"], [pk])
        ts(DVE, rr, pt[:], 1024.0 * EPS, ALU.add, [pk], ["frr"])
        act(rr, rr, AF.Ln, ["frr"], ["frr"])
        act(rr, rr, AF.Exp, ["frr"], ["frr"], scale=-0.5)
        for kc in range(8):
            oc = FIN["oc"]
            nob = len(OUTB)
            ob = OUTB[oc % nob]
            obk = ("outb", oc % nob)
            FIN["oc"] = oc + 1
            stt(ob, XT[:, kc, tsl], fcol[:, kc:kc + 1], rr, ALU.mult, ALU.mult, [("x", kc, p), "frr", "cols"], [obk])
            ts(DVE, ob, ob, 32.0, ALU.mult, [obk], [obk])
            P.dma(SP, outT_d[kc * 128:(kc + 1) * 128, tsl], ob, reads=[obk], chan=obk)

    def ffn_layer(l):
        barrier()
        ar = Arena()
        H2 = ar.bf(8 * T).rearrange("p (k t) -> p k t", k=8)
        for p in range(NP):
            ar2 = Arena()
            ar2.off = 8 * T // 2
            rmsnorm_mod(l, None, p, A2, 24, lambda kc, p=p: H2[:, kc, p * TB:(p + 1) * TB], ("h2", p), ar2, "n2")
        ar.off = 8 * T // 2 + 2048
        AT = [ar.bf(4 * TB).rearrange("p (k t) -> p k t", k=4) for _ in range(2)]
        SG = [ar.f32(TB) for _ in range(2)]
        moe = (l % 2 == 1)
        if moe:
            CB = ar.f32(T)
            LG = ar.f32(16 * 8)
            CMB = ar.f32(16 * 8)
            TMP8 = ar.f32(16 * 8)
            MX = ar.f32(32)
            RWB = ar.bf(8 * 8).rearrange("p (k n) -> p k n", k=8)
            P.dma(POOL, RWB, router_w[0].rearrange("(kc p) n -> p kc n", p=128), writes=["rwb"], chan="rwb")
            lg3 = LG.rearrange("p (t e) -> p t e", e=8)
            cm3 = CMB.rearrange("p (t e) -> p t e", e=8)
            tp3 = TMP8.rearrange("p (t e) -> p t e", e=8)
            pl, plk = psum()
            for tt_ in range(16):
                mm(pl[:, tt_ * 8:(tt_ + 1) * 8], [(H2[:, kc, tt_ * 128:(tt_ + 1) * 128], RWB[:, kc, :]) for kc in range(8)],
                   [("h2", tt_ // 4), "rwb"], [plk])
            tt(DVE, lg3, pl[:, 0:128].rearrange("p (t e) -> p t e", e=8), RB[:, 0:8].unsqueeze(1).to_broadcast([128, 16, 8]),
               ALU.add, [plk, "rb"], ["lg"])
            m1 = MX[:, 0:16]
            m2 = MX[:, 16:32]
            P.op(DVE, lambda e: e.tensor_reduce(out=m1, in_=lg3, axis=mybir.AxisListType.X, op=ALU.max), ["lg"], ["mx1"])
            tt(DVE, tp3, lg3, m1.unsqueeze(2).to_broadcast([128, 16, 8]), ALU.is_ge, ["lg", "mx1"], ["tp"])
            stt(tp3, tp3, -1e30, lg3, ALU.mult, ALU.add, ["tp", "lg"], ["tp"])
            P.op(DVE, lambda e: e.tensor_reduce(out=m2, in_=tp3, axis=mybir.AxisListType.X, op=ALU.max), ["tp"], ["mx2"])
            tt(DVE, tp3, lg3, m2.unsqueeze(2).to_broadcast([128, 16, 8]), ALU.is_ge, ["lg", "mx2", "tp"], ["tp"])
            tt(DVE, cm3, lg3, m1.unsqueeze(2).to_broadcast([128, 16, 8]), ALU.subtract, ["lg", "mx1"], ["cmb"])
            act(CMB, CMB, AF.Exp, ["cmb"], ["cmb"])
            tt(DVE, cm3, cm3, tp3, ALU.mult, ["cmb", "tp"], ["cmb"])
            tt(DVE, m2, m2, m1, ALU.subtract, ["mx1", "mx2"], ["mx2"])
            act(m2, m2, AF.Exp, ["mx2"], ["mx2"])
            ts(DVE, m2, m2, 1.0, ALU.add, ["mx2"], ["mx2"])
            P.op(DVE, lambda e: e.reciprocal(out=m2, in_=m2), ["mx2"], ["mx2"])
            tt(DVE, cm3, cm3, m2.unsqueeze(2).to_broadcast([128, 16, 8]), ALU.mult, ["cmb", "mx2"], ["cmb"])
            RID = [ar.f32(128) for _ in range(2)]
            blocks = [(e, f0, 512) for e in range(NEXP) for f0 in range(0, DFE, 512)]
            wg_d, wu_d, wd_d = moe_g[0], moe_u[0], moe_d[0]
        else:
            blocks = [(None, f0, min(512, DFF - f0)) for f0 in range(0, DFF, 512)]
            wg_d, wu_d, wd_d = ffn_g[0], ffn_u[0], ffn_d[0]
        fb = None
        if final and l == layers[-1]:
            fb = dict(sq=[ar.bf(TB) for _ in range(2)], rr=ar.f32(TB), OUTB=[ar.f32(TB) for _ in range(2)])
            FIN["done"] = True
        cur_e = [None]
        it = 0
        def issue_block(bi):
            e_, f0, F = blocks[bi]
            nfc = F // 128
            s_g, s_u, s_d = wslot(1), wslot(1), wslot(1)
            wg = wview(s_g, "p (k n) -> p k n", k=8)
            wu = wview(s_u, "p (k n) -> p k n", k=8)
            wd = wview(s_d, "p (k n) -> p k n", k=4)
            gsrc = wg_d if e_ is None else wg_d[e_]
            usrc = wu_d if e_ is None else wu_d[e_]
            dsrc = wd_d if e_ is None else wd_d[e_]
            wdma(wg[:, :, 0:F], gsrc[:, f0:f0 + F].rearrange("(kc p) n -> p kc n", p=128), s_g)
            wdma(wu[:, :, 0:F], usrc[:, f0:f0 + F].rearrange("(kc p) n -> p kc n", p=128), s_u)
            wdma(wd[:, 0:nfc, :], dsrc[f0:f0 + F, :].rearrange("(fc p) n -> p fc n", p=128), s_d)
            return (s_g, s_u, s_d, wg, wu, wd)

        wslot_ctr[0] += (-wslot_ctr[0]) % 3
        issued = {0: issue_block(0)}
        if len(blocks) > 1:
            issued[1] = issue_block(1)
        items = [(bi, p) for bi in range(len(blocks)) for p in range(NP)]

        def cb_prep(e_):
            for p in range(NP):
                pc, pck = psum()
                for i4 in range(4):
                    tt_ = p * 4 + i4
                    rid = RID[tt_ % 2]
                    ts(DVE, rid, ident, CMB[:, tt_ * 8 + e_:tt_ * 8 + e_ + 1], ALU.mult, ["cmb", "con"], [("rid", tt_ % 2)])
                    mm(pc[:, i4 * 128:(i4 + 1) * 128], [(CON[:, C_ONES:C_ONES + 128], rid)], [("rid", tt_ % 2), "con"], [pck])
                cp(ACT, CB[:, p * TB:(p + 1) * TB], pc[:], [pck], [("cb", p)])

        def gu(k):
            bi, p = items[k]
            e_, f0, F = blocks[bi]
            nfc = F // 128
            s_g, s_u, s_d, wg, wu, wd = issued[bi]
            if moe and cur_e[0] != e_:
                cur_e[0] = e_
                cb_prep(e_)
            tsl = slice(p * TB, (p + 1) * TB)
            at = AT[k % 2]
            atk = ("at", k % 2)
            for fc in range(nfc):
                pgt, pgk_ = psum()
                put, puk_ = psum()
                fs = slice(fc * 128, (fc + 1) * 128)
                mm(pgt[:], [(wg[:, kc, fs], H2[:, kc, tsl]) for kc in range(8)], wkeys(s_g) + [("h2", p)], [pgk_])
                mm(put[:], [(wu[:, kc, fs], H2[:, kc, tsl]) for kc in range(8)], wkeys(s_u) + [("h2", p)], [puk_])
                sg = SG[fc % 2]
                sgk = ("sg", fc % 2)
                act(sg, pgt[:], AF.Silu, [pgk_], [sgk])
                if moe:
                    tt(POOL, sg, sg, CB[:, tsl], ALU.mult, [sgk, ("cb", p)], [sgk])
                tt(DVE, at[:, fc, :], put[:], sg, ALU.mult, [puk_, sgk], [atk])

        def down(k):
            bi, p = items[k]
            e_, f0, F = blocks[bi]
            nfc = F // 128
            s_g, s_u, s_d, wg, wu, wd = issued[bi]
            tsl = slice(p * TB, (p + 1) * TB)
            at = AT[k % 2]
            atk = ("at", k % 2)
            for dc in range(8):
                pt, pk = psum()
                mm(pt[:], [(wd[:, fc, dc * 128:(dc + 1) * 128], at[:, fc, :]) for fc in range(nfc)],
                   wkeys(s_d) + [atk], [pk])
                stt(XT[:, dc, tsl], pt[:], CUR["mod"][:, 40 + dc:40 + dc + 1], XT[:, dc, tsl], ALU.mult, ALU.add,
                    [pk, "modc", ("x", dc, p)], [("x", dc, p)])
            if fb is not None and bi == len(blocks) - 1:
                final_norm_pass(p, fb)
            if p == NP - 1:
                issued.pop(bi)
                if bi + 2 < len(blocks):
                    issued[bi + 2] = issue_block(bi + 2)

        gu(0)
        for k in range(len(items)):
            if k + 1 < len(items):
                gu(k + 1)
            down(k)

    def chk(n):
        if stage <= n:
            raise _Stop()

    try:
        chk(1)
        for li_, l in enumerate(layers):
            CUR["mod"] = MODS[l]
            if li_ == 0:
                ada_blocks_now(l, [0, 1, 2, 3])
                derive_early(l)
            else:
                derive_early(l)
                derive_late(l)
            chk(2)
            mixer_layer(l)
            if ffn:
                ffn_layer(l)
    except _Stop:
        pass

    if final and FIN["done"]:
        fw = [("outb", i) for i in range(2)]
    elif final:
        barrier()
        ar = Arena()
        fb = dict(sq=[ar.bf(TB) for _ in range(2)], rr=ar.f32(TB), OUTB=[ar.f32(TB) for _ in range(4)])
        for p in range(NP):
            final_norm_pass(p, fb)
        fw = [("outb", i) for i in range(4)]
    else:
        for kc in range(8):
            P.dma(SP, outT_d[kc * 128:(kc + 1) * 128, :], XT[:, kc, :], reads=[("x", kc, p) for p in range(NP)],
                  chan=("xstore", kc))
        fw = [("xstore", kc) for kc in range(8)]
    P.emit(st, final_waits=fw)
    return st


def make_consts():
    c = np.zeros((128, NCONST), np.float32)
    c[:, C_ID:C_ID + 128] = np.eye(128, dtype=np.float32)
    s = np.arange(128)[:, None]
    t = np.arange(128)[None, :]
    c[:, C_MASK:C_MASK + 128] = np.where(s > t, np.float32(1e30), np.float32(0.0))
    c[:, C_ONES:C_ONES + 128] = 1.0
    rm = np.ones(512, np.float32)
    rm[0] = 0.0
    ra = np.zeros(512, np.float32)
    ra[0] = -1e30
    c[0:4, C_RMUL:C_RMUL + 512] = rm[None, :]
    c[0:4, C_RADD:C_RADD + 512] = ra[None, :]
    for g, win in enumerate((2, 4, 8, 16)):
        tt = np.arange(16)
        c[:, C_INVC + g * 16:C_INVC + (g + 1) * 16] = (1.0 / np.minimum(tt + 1, win)).astype(np.float32)[None, :]
    return c


def col(v):
    v = np.asarray(v, np.float32)
    return np.ascontiguousarray(v.reshape(-1, 128).T)


def make_cols(norm_mix, norm_ffn, b_ada, conv_w, pool_scale, final_norm, head_gain):
    c = np.zeros((128, NCOLS), np.float32)
    for l in range(2):
        o = l * LC
        c[:, o:o + 8] = col(norm_mix[l])
        c[:, o + 8:o + 16] = col(norm_ffn[l])
        c[:, o + 16:o + 64] = col(b_ada[l])
        cw = np.asarray(conv_w[l], np.float32)
        cc = cw.reshape(4, 16, 128).transpose(2, 1, 0).reshape(128, 64)
        c[:, o + 64:o + 128] = cc
        c[:, o + 128:o + 132] = col(pool_scale[l])
        c[:, o + 132:o + 140] = col(head_gain[l])
    c[:, 2 * LC:2 * LC + 8] = col(final_norm)
    return c


_CACHE = {}


def kernel(x, c, norm_mix, norm_ffn, w_ada, b_ada, w_in, conv_w, i_bias, f_bias, head_gain,
           pool_w, pool_scale, proj_a, proj_b, w_out, ffn_w_gate, ffn_w_up, ffn_w_down,
           router_w, router_b, moe_w_gate, moe_w_up, moe_w_down, final_norm, _layers=(0, 1), _final=True, _ffn=True, _stage=99):
    f = lambda a: np.ascontiguousarray(np.asarray(a, dtype=np.float32))
    x = f(x)
    c = f(c)
    nc = bass.Bass("TRN2", target_bir_lowering=False)
    st = build(nc, layers=_layers, final=_final, ffn=_ffn, stage=_stage)
    consts = make_consts()
    cols = make_cols(f(norm_mix), f(norm_ffn), f(b_ada), f(conv_w), f(pool_scale), f(final_norm), f(head_gain))
    gb = np.stack([f(i_bias)[0], f(f_bias)[0], f(i_bias)[1], f(f_bias)[1]], axis=1).astype(np.float32)
    rb = np.ascontiguousarray(np.broadcast_to(f(router_b)[0][None, :], (128, 8))).astype(np.float32)
    shared = dict(cols=cols, gb=np.ascontiguousarray(gb), rb=rb, consts=consts,
                  w_ada=f(w_ada), w_in=f(w_in), pool_w=f(pool_w), proj_a=f(proj_a), proj_b=f(proj_b),
                  w_out=f(w_out), ffn_w_gate=f(ffn_w_gate), ffn_w_up=f(ffn_w_up), ffn_w_down=f(ffn_w_down),
                  router_w=f(router_w))
    if 1 in _layers:
        shared.update(moe_w_gate=f(moe_w_gate), moe_w_up=f(moe_w_up), moe_w_down=f(moe_w_down))
    in_maps = []
    for b in range(8):
        m = dict(shared)
        m["xT"] = np.ascontiguousarray(x[b].T)
        m["cT"] = col(c[b])
        in_maps.append(m)
    with st:
        pass
    res = run_bass_kernel_spmd(nc, in_maps, core_ids=list(range(8)))
    out = np.stack([np.ascontiguousarray(res.results[b]["outT"].T) for b in range(8)], axis=0)
    return out.astype(np.float32)
```
